# Optimizing a Trainium2 kernel written in Bass

```python
import math
import jax, jax.numpy as jnp
from jax import lax
import numpy as np

D_MODEL = 2048
BATCH = 1
SEQ = 16384
DEPTH = 2

CHUNK = 64
Q_BLOCK = 128
HEAD_DIM = 128
N_HEADS_A = 6
N_HEADS_B = 6
N_HEADS_C = 4
WIDTH_A = N_HEADS_A * HEAD_DIM
WIDTH_B = N_HEADS_B * HEAD_DIM
WIDTH_C = N_HEADS_C * HEAD_DIM
DIFF_DIM = HEAD_DIM // 2
LEFT_CHUNKS = 8
BAND_CHUNKS = LEFT_CHUNKS + 1
REL_CLIP = 256
ROPE_THETA = 500000.0
ROPE_DIM = DIFF_DIM // 4
D_FF = 7168
N_EXPERTS = 8
TOP_K = 2
N_DENSE = (DEPTH + 1) // 2
N_MOE = DEPTH // 2
EPS = 1e-6
NEG_INF = -1e30
IN_WIDTHS = (WIDTH_A, WIDTH_A, WIDTH_A, N_HEADS_A,
             WIDTH_B, WIDTH_B, WIDTH_B,
             WIDTH_C, WIDTH_C, WIDTH_C)
D_IN = 3 * (WIDTH_A + WIDTH_B + WIDTH_C) + N_HEADS_A

kernel_name = "hybrid_fox_chunkband_diffattn_sandwich_adaln_moe"


def rms_norm(x, g):
    xf = x.astype(jnp.float32)
    y = xf * lax.rsqrt(jnp.mean(xf * xf, axis=-1, keepdims=True) + EPS)
    return (y * g.astype(jnp.float32)).astype(x.dtype)


def partial_rope(x, positions):
    half = ROPE_DIM // 2
    inv = ROPE_THETA ** (-(jnp.arange(half, dtype=jnp.float32) * 2.0 / ROPE_DIM))
    ang = positions.astype(jnp.float32)[..., None] * inv
    cos = jnp.cos(ang)[:, :, None, None, :]
    sin = jnp.sin(ang)[:, :, None, None, :]
    xr = x[..., :ROPE_DIM].astype(jnp.float32)
    x1, x2 = xr[..., :half], xr[..., half:]
    rot = jnp.concatenate([x1 * cos - x2 * sin, x2 * cos + x1 * sin], axis=-1)
    return jnp.concatenate([rot.astype(x.dtype), x[..., ROPE_DIM:]], axis=-1)


def forgetting_attention(q, k, v, log_f):
    B, S, H, d = q.shape
    nb = S // Q_BLOCK
    F = jnp.cumsum(log_f, axis=1)
    Ft = F.transpose(0, 2, 1)
    qb = q.reshape(B, nb, Q_BLOCK, H, d).transpose(1, 0, 2, 3, 4)
    Fb = F.reshape(B, nb, Q_BLOCK, H).transpose(1, 0, 3, 2)
    kpos = jnp.arange(S)
    scale = d ** -0.5

    def block(args):
        qi, Fi, i = args
        s = jnp.einsum('bqhd,bkhd->bhqk', qi, k, preferred_element_type=jnp.float32) * scale
        s = s + Fi[..., None] - Ft[:, :, None, :]
        qpos = i * Q_BLOCK + jnp.arange(Q_BLOCK)
        s = jnp.where(kpos[None, :] <= qpos[:, None], s, NEG_INF)
        p = jax.nn.softmax(s, axis=-1)
        return jnp.einsum('bhqk,bkhd->bqhd', p.astype(v.dtype), v)

    out = lax.map(block, (qb, Fb, jnp.arange(nb)))
    return out.transpose(1, 0, 2, 3, 4).reshape(B, S, H, d)


def chunk_band_attention(q, k, v, rel_bias):
    B, S, H, d = q.shape
    nc = S // CHUNK
    band = BAND_CHUNKS * CHUNK
    pad = ((0, 0), (LEFT_CHUNKS * CHUNK, 0), (0, 0), (0, 0))
    kp = jnp.pad(k, pad).reshape(B, nc + LEFT_CHUNKS, CHUNK, H, d)
    vp = jnp.pad(v, pad).reshape(B, nc + LEFT_CHUNKS, CHUNK, H, d)
    k_band = jnp.concatenate([kp[:, j:j + nc] for j in range(BAND_CHUNKS)], axis=2)
    v_band = jnp.concatenate([vp[:, j:j + nc] for j in range(BAND_CHUNKS)], axis=2)
    qc = q.reshape(B, nc, CHUNK, H, d)
    s = jnp.einsum('bnqhd,bnkhd->bnhqk', qc, k_band, preferred_element_type=jnp.float32) * (d ** -0.5)
    s_rel = jnp.arange(band) - LEFT_CHUNKS * CHUNK
    dist = jnp.arange(CHUNK)[:, None] - s_rel[None, :]
    idx = jnp.clip(dist, -REL_CLIP, REL_CLIP) + REL_CLIP
    s = s + rel_bias[:, idx].astype(jnp.float32)
    valid = (jnp.arange(nc)[:, None] * CHUNK + s_rel[None, :]) >= 0
    s = jnp.where(valid[None, :, None, None, :], s, NEG_INF)
    p = jax.nn.softmax(s, axis=-1)
    o = jnp.einsum('bnhqk,bnkhd->bnqhd', p.astype(v.dtype), v_band)
    return o.reshape(B, S, H, d)


def diff_attention(q, k, v, lam):
    B, S, H, _, dd = q.shape
    nb = S // Q_BLOCK
    qb = q.reshape(B, nb, Q_BLOCK, H, 2, dd).transpose(1, 0, 2, 3, 4, 5)
    kchunk = jnp.arange(S) // CHUNK
    scale = dd ** -0.5

    def block(args):
        qi, i = args
        s = jnp.einsum('bqhnd,bkhnd->bnhqk', qi, k, preferred_element_type=jnp.float32) * scale
        qchunk = (i * Q_BLOCK + jnp.arange(Q_BLOCK)) // CHUNK
        s = jnp.where(kchunk[None, :] <= qchunk[:, None], s, NEG_INF)
        p = jax.nn.softmax(s, axis=-1)
        w = p[:, 0] - lam * p[:, 1]
        return jnp.einsum('bhqk,bkhd->bqhd', w.astype(v.dtype), v)

    out = lax.map(block, (qb, jnp.arange(nb)))
    return out.transpose(1, 0, 2, 3, 4).reshape(B, S, H, v.shape[-1])


def hybrid_mixer(h, positions, w_in, b_f, rel_bias, lam, onorm, w_o, layer_idx):
    B, S, _ = h.shape
    proj = jnp.einsum('bsd,de->bse', h, w_in)
    split_pts = np.cumsum(np.array(IN_WIDTHS))[:-1].tolist()
    qa, ka, va, fa, qb, kb, vb, qc, kc, vc = jnp.split(proj, split_pts, axis=-1)
    heads = lambda t, n: t.reshape(B, S, n, HEAD_DIM)
    log_f = jax.nn.log_sigmoid(fa.astype(jnp.float32) + b_f.astype(jnp.float32))
    o_a = forgetting_attention(heads(qa, N_HEADS_A), heads(ka, N_HEADS_A), heads(va, N_HEADS_A), log_f)
    o_b = chunk_band_attention(heads(qb, N_HEADS_B), heads(kb, N_HEADS_B), heads(vb, N_HEADS_B), rel_bias)
    qc = partial_rope(qc.reshape(B, S, N_HEADS_C, 2, DIFF_DIM), positions)
    kc = partial_rope(kc.reshape(B, S, N_HEADS_C, 2, DIFF_DIM), positions)
    lam_init = 0.8 - 0.6 * math.exp(-0.3 * layer_idx)
    lf = lam.astype(jnp.float32)
    lam_full = jnp.exp(jnp.sum(lf[0] * lf[1])) - jnp.exp(jnp.sum(lf[2] * lf[3])) + lam_init
    o_c = diff_attention(qc, kc, heads(vc, N_HEADS_C), lam_full)
    o = jnp.concatenate([rms_norm(o_a, onorm[0]),
                         rms_norm(o_b, onorm[1]),
                         rms_norm(o_c, onorm[2]) * (1.0 - lam_init)], axis=2)
    return jnp.einsum('bse,ed->bsd', o.reshape(B, S, -1), w_o)


def swiglu(h, wg, wu, wd):
    a = jnp.einsum('bsd,df->bsf', h, wg)
    u = jnp.einsum('bsd,df->bsf', h, wu)
    return jnp.einsum('bsf,fd->bsd', jax.nn.silu(a) * u, wd)


def moe_ffn(h, router_w, router_b, wg, wu, wd):
    logits = jnp.einsum('bsd,de->bse', h, router_w).astype(jnp.float32) + router_b.astype(jnp.float32)
    top_v, top_i = lax.top_k(logits, TOP_K)
    gates = jax.nn.softmax(top_v, axis=-1)
    combine = jnp.sum(jax.nn.one_hot(top_i, N_EXPERTS, dtype=jnp.float32) * gates[..., None], axis=-2)
    out = jnp.zeros(h.shape, jnp.float32)
    for e in range(N_EXPERTS):
        out = out + combine[..., e:e + 1] * swiglu(h, wg[e], wu[e], wd[e]).astype(jnp.float32)
    return out.astype(h.dtype)


def setup_inputs(seed: int = 0) -> dict:
    key = jax.random.key(seed)
    ks = jax.random.split(key, 21)
    f32 = jnp.float32
    D = D_MODEL
    nrm = lambda k, shape, scale: jax.random.normal(k, shape, f32) * scale
    x = nrm(ks[0], (BATCH, SEQ, D), 1.0)
    c = nrm(ks[1], (BATCH, D), 1.0)
    offset = jax.random.randint(ks[2], (BATCH, 1), 0, 1024, dtype=jnp.int32) * CHUNK
    positions = offset + jnp.arange(SEQ, dtype=jnp.int32)[None, :]
    return {
        "x": x,
        "c": c,
        "positions": positions,
        "mod_w": nrm(ks[3], (DEPTH, D, 6 * D), 0.5 * D ** -0.5),
        "mod_b": nrm(ks[4], (DEPTH, 6 * D), 0.02),
        "norm_g": 1.0 + nrm(ks[5], (DEPTH, 4, D), 0.02),
        "w_in": nrm(ks[6], (DEPTH, D, D_IN), D ** -0.5),
        "b_f": nrm(ks[7], (DEPTH, N_HEADS_A), 0.5),
        "rel_bias": nrm(ks[8], (DEPTH, N_HEADS_B, 2 * REL_CLIP + 1), 0.5),
        "lam": nrm(ks[9], (DEPTH, 4, DIFF_DIM), 0.1),
        "onorm": 1.0 + nrm(ks[10], (DEPTH, 3, HEAD_DIM), 0.02),
        "w_o": nrm(ks[11], (DEPTH, D, D), D ** -0.5),
        "ffn_wg": nrm(ks[12], (N_DENSE, D, D_FF), D ** -0.5),
        "ffn_wu": nrm(ks[13], (N_DENSE, D, D_FF), D ** -0.5),
        "ffn_wd": nrm(ks[14], (N_DENSE, D_FF, D), D_FF ** -0.5),
        "router_w": nrm(ks[15], (N_MOE, D, N_EXPERTS), D ** -0.5),
        "router_b": nrm(ks[16], (N_MOE, N_EXPERTS), 0.01),
        "exp_wg": nrm(ks[17], (N_MOE, N_EXPERTS, D, D_FF), D ** -0.5),
        "exp_wu": nrm(ks[18], (N_MOE, N_EXPERTS, D, D_FF), D ** -0.5),
        "exp_wd": nrm(ks[19], (N_MOE, N_EXPERTS, D_FF, D), D_FF ** -0.5),
    }


def reference(x, c, positions, mod_w, mod_b, norm_g, w_in, b_f, rel_bias, lam, onorm, w_o,
              ffn_wg, ffn_wu, ffn_wd, router_w, router_b, exp_wg, exp_wu, exp_wd):
    cond = jax.nn.silu(c)
    for l in range(DEPTH):
        mod = (jnp.einsum('bd,de->be', cond, mod_w[l]) + mod_b[l])[:, None, :]
        sh_m, sc_m, g_m, sh_f, sc_f, g_f = jnp.split(mod, 6, axis=-1)
        h = rms_norm(x, norm_g[l, 0]) * (1.0 + sc_m) + sh_m
        y = hybrid_mixer(h, positions, w_in[l], b_f[l], rel_bias[l], lam[l], onorm[l], w_o[l], l)
        x = x + g_m * rms_norm(y, norm_g[l, 1])
        h = rms_norm(x, norm_g[l, 2]) * (1.0 + sc_f) + sh_f
        i = l // 2
        if l % 2 == 0:
            y = swiglu(h, ffn_wg[i], ffn_wu[i], ffn_wd[i])
        else:
            y = moe_ffn(h, router_w[i], router_b[i], exp_wg[i], exp_wu[i], exp_wd[i])
        x = x + g_f * rms_norm(y, norm_g[l, 3])
    return x
```

```python
import math
import numpy as np
import concourse.bass as bass
import concourse.mybir as mybir
from concourse.bass_utils import run_bass_kernel_spmd
from contextlib import ExitStack

F32, BF16, I32 = mybir.dt.float32, mybir.dt.bfloat16, mybir.dt.int32
AF = mybir.ActivationFunctionType
ALU = mybir.AluOpType
AX = mybir.AxisListType

import os
SKIP = set(os.environ.get('KSKIP', '').split(','))
NCORES = 8
D = 2048
S = 16384
TOK = S // NCORES
KC = D // 128
DFF = 7168
FC = DFF // 128
NE = 8
NH_A, NH_B, NH_C = 6, 6, 4
NHEAD = 16
EPS = 1e-6
REL_CLIP = 256
NFM = 16 * 256 + 4 * 256
ELEN = 1535


def own_tiles(r):
    return [r, 15 - r, 16 + r, 31 - r]


def tile_loc(g):
    if g < 8:
        return g, 0
    if g < 16:
        return 15 - g, 1
    if g < 24:
        return g - 16, 2
    return 31 - g, 3


class Op:
    __slots__ = ("eng", "fn", "deps", "is_dma", "sem", "val", "signal", "sigval", "idx", "raw")


class Sched:
    ENGS = ("pe", "act", "dve", "pool", "sp")
    NSLOT = 12

    def __init__(self, nc, es):
        self.nc = nc
        self.ops = {e: [] for e in self.ENGS}
        self.last_w = {}
        self.readers = {}
        self.esem = {e: es.enter_context(nc.semaphore("s_" + e)) for e in self.ENGS}
        self.slots = {q: [es.enter_context(nc.semaphore("d_%s%d" % (q, i))) for i in range(self.NSLOT)]
                      for q in ("sp", "pool", "act")}
        self.slot_n = {q: 0 for q in self.slots}
        self.slot_last = {q: [None] * self.NSLOT for q in self.slots}
        self.es = es
        self.ncc = 0
        self.bar_gen = 0
        self.bar_deps = []
        self.eng_gen = {e: 0 for e in self.ENGS}

    def barrier(self):
        deps = []
        for e in self.ENGS:
            if self.ops[e]:
                last = None
                for o in reversed(self.ops[e]):
                    if not o.is_dma:
                        last = o
                        break
                if last is not None:
                    deps.append(last)
        for q in self.slots:
            for o in self.slot_last[q]:
                if o is not None:
                    deps.append(o)
        deps += [o for o in getattr(self, "cc_ops", [])]
        self.bar_deps = deps
        self.bar_gen += 1

    def add(self, eng, fn, reads=(), writes=(), dma=False, cc=False):
        o = Op()
        o.eng, o.fn, o.is_dma, o.signal, o.sigval = eng, fn, (dma or cc), False, None
        deps = {}
        for k in reads:
            for w in self.last_w.get(k, ()):
                deps[id(w)] = (w, True)
        for k in writes:
            for w in self.last_w.get(k, ()):
                if id(w) not in deps and not (w.is_dma and (dma or cc) and not self.readers.get(k)):
                    deps[id(w)] = (w, False)
            for rd in self.readers.get(k, {}).values():
                for r_ in (rd if isinstance(rd, list) else [rd]):
                    if id(r_) not in deps:
                        deps[id(r_)] = (r_, False)
        if dma:
            q = eng
            n = self.slot_n[q]
            self.slot_n[q] = n + 1
            sl = n % self.NSLOT
            prev = self.slot_last[q][sl]
            if prev is not None:
                deps[id(prev)] = (prev, False)
            self.slot_last[q][sl] = o
            o.sem = self.slots[q][sl]
            o.val = 16 * (n // self.NSLOT + 1)
        elif cc:
            o.sem = self.es.enter_context(self.nc.semaphore("cc%d" % self.ncc))
            self.ncc += 1
            o.val = 1
        if self.eng_gen[eng] < self.bar_gen:
            self.eng_gen[eng] = self.bar_gen
            for d_ in self.bar_deps:
                if id(d_) not in deps:
                    deps[id(d_)] = (d_, True)
        if cc:
            self.cc_ops = getattr(self, "cc_ops", []) + [o]
        final = []
        for (d, raw) in deps.values():
            if d is o:
                continue
            if (not d.is_dma) and (not o.is_dma) and d.eng == eng:
                if eng == "pe" or not raw:
                    continue
            final.append(d)
            if not d.is_dma:
                d.signal = True
        o.deps = final
        for k in writes:
            prev = self.last_w.get(k, [])
            if o.is_dma and prev and prev[-1].is_dma and not self.readers.get(k):
                prev.append(o)
            else:
                self.last_w[k] = [o]
            self.readers[k] = {}
        for k in reads:
            rd = self.readers.setdefault(k, {})
            if o.is_dma:
                rd.setdefault("dma", []).append(o)
            else:
                rd[eng] = o
        o.idx = len(self.ops[eng])
        self.ops[eng].append(o)
        return o

    def emit(self, block):
        for e in self.ENGS:
            c = 0
            for o in self.ops[e]:
                if o.signal:
                    c += 1
                    o.sigval = c
        sched = self

        def run(engname, h):
            waited = {}
            for o in sched.ops[engname]:
                red = {}
                for d in o.deps:
                    kk = ("D", id(d.sem)) if d.is_dma else ("E", d.eng)
                    vv = d.val if d.is_dma else d.sigval
                    if kk not in red or vv > red[kk][0]:
                        red[kk] = (vv, d)
                for (vv, d) in red.values():
                    if d.is_dma:
                        key = ("D", id(d.sem))
                        v = d.val
                        sem = d.sem
                    else:
                        key = ("E", d.eng)
                        v = d.sigval
                        sem = sched.esem[d.eng]
                    if waited.get(key, 0) >= v:
                        continue
                    waited[key] = v
                    h.wait_ge(sem, v)
                ins = o.fn(h)
                if o.is_dma:
                    ins.then_inc(o.sem, 16 if o.val % 16 == 0 and o.val >= 16 and not _is_cc(o) else 1)
                elif o.signal:
                    ins.then_inc(sched.esem[engname], 1)

        @block.tensor
        def _(h):
            run("pe", h)

        @block.scalar
        def _(h):
            run("act", h)

        @block.vector
        def _(h):
            run("dve", h)

        @block.gpsimd
        def _(h):
            run("pool", h)

        @block.sync
        def _(h):
            run("sp", h)


def _is_cc(o):
    return o.val == 1


class Builder:
    def __init__(self, stop_after=None, taps=()):
        self.stop_after = stop_after
        self.taps = taps
        self.nc = bass.Bass("TRN2", target_bir_lowering=False)
        self.es = ExitStack()
        self.uid = 0

    def din(self, name, shape, dt=F32):
        if not hasattr(self, "in_names"):
            self.in_names = []
        if name in ("fwg", "fwu", "fwd") and self.stop_after in ("P0", "A0", "O0", "W"):
            return None
        if name in ("ewg", "ewu", "ewd") and self.stop_after is not None:
            return None
        self.in_names.append(name)
        return self.nc.dram_tensor(name, list(shape), dt, kind="ExternalInput")

    def dint(self, name, shape, dt=BF16):
        return self.nc.dram_tensor(name, list(shape), dt)

    def sb(self, es, name, shape, dt):
        esz = 2 if dt == BF16 else 4
        n = 1
        for s_ in shape[1:]:
            n *= s_
        nb = (n * esz + 63) // 64 * 64
        off = self.sb_off
        self.sb_off += nb
        self.sb_peak = max(self.sb_peak, self.sb_off)
        assert self.sb_off <= self.SB_BYTES, ("SBUF overflow", name, self.sb_off)
        v = self.big[0:shape[0], off // 2:(off + n * esz) // 2]
        if dt != BF16:
            v = v.bitcast(dt)
        if len(shape) == 3:
            v = v.rearrange("p (a b) -> p a b", a=shape[1])
        return v

    def scope(self):
        b = self

        class _S:
            def __enter__(s):
                s.mark = b.sb_off
                return s

            def __exit__(s, *a):
                b.sb_off = s.mark
                b.sc.barrier()
                return False
        return _S()

    def build(self):
        nc = self.nc
        es = self.es
        with es:
            self.sc = Sched(nc, es)
            self.SB_BYTES = 190 * 1024
            self.big = es.enter_context(nc.sbuf_tensor("big", [128, self.SB_BYTES // 2], BF16))
            self.sb_off = 0
            self.sb_peak = 0
            self.declare()
            self.ps = [es.enter_context(nc.psum_tensor("ps%d" % i, [128, 512], F32)) for i in range(8)]
            self.consts()
            self.weights_prep()
            self.mod_phase()
            if self.stop_after == "W":
                self.tap("mod", self.mod_f, ("mod_f",))
                self.tap("wo", self.wo_f[1], ("wo1",))
                self.done = True
            for l in range(2):
                if self.done:
                    break
                self.layer(l)
            self.finish()
            with nc.Block() as block:
                self.sc.emit(block)
        return nc

    def declare(self):
        d = self.din
        self.x_in = d("x", [TOK, D])
        self.pos_in = d("pos", [1, TOK], I32)
        self.c_in = d("c", [D, 1])
        self.modw_in = d("modw", [2, D, 1536])
        self.modb_in = d("modb", [1, 3072])
        self.normg_in = d("normg", [8, D])
        self.wfm_in = d("wfm", [2, 256, NFM])
        self.wv_in = d("wv", [2, 256, D])
        self.wgate_in = d("wgate", [2, D, 8])
        self.bf_in = d("bf", [2, 8])
        self.relE_in = d("relE", [2, 6, ELEN])
        self.lam_in = d("lam", [2, 256])
        self.onorm_in = d("onorm", [2, 3, 128])
        self.wo_in = d("wo", [2, 256, D])
        self.fwg_in = d("fwg", [256, DFF])
        self.fwu_in = d("fwu", [256, DFF])
        self.fwd_in = d("fwd", [DFF // 8, D])
        self.rw_in = d("rw", [D, 8])
        self.rb_in = d("rb", [1, 8])
        self.ewg_in = d("ewg", [D, DFF])
        self.ewu_in = d("ewu", [D, DFF])
        self.ewd_in = d("ewd", [DFF, D])
        self.cst_in = d("cst", [128, 4 * 128])
        self.cmask_in = d("cmask", [128, 8 * 512])
        self.bmask_in = d("bmask", [128, 8 * 512])
        self.tb_in = d("tb", [128, 256])
        self.sel_in = d("sel", [8, 128])
        self.out = self.nc.dram_tensor("out", [TOK, D], F32, kind="ExternalOutput")
        t = self.dint
        self.wfm_b = [t("wfm_b%d" % l, [256, NFM]) for l in range(2)]
        self.wfm_f = [t("wfm_f%d" % l, [D, NFM]) for l in range(2)]
        self.wv_b = [t("wv_b%d" % l, [256, D]) for l in range(2)]
        self.wv_f = [t("wv_f%d" % l, [D, D]) for l in range(2)]
        self.wo_b = [t("wo_b%d" % l, [256, D]) for l in range(2)]
        self.wo_f = [t("wo_f%d" % l, [D, D]) for l in range(2)]
        self.fw_b = [t("fwg_b", [256, DFF]), t("fwu_b", [256, DFF]), t("fwd_b", [DFF // 8, D])]
        self.fw_f = [t("fwg_f", [D, DFF]), t("fwu_f", [D, DFF]), t("fwd_f", [DFF, D])]
        self.ew_b = [t("ewg_b", [D, DFF]), t("ewu_b", [D, DFF]), t("ewd_b", [DFF, D])]
        self.ew_f = [t("ewg_f", [NE * D, DFF]), t("ewu_f", [NE * D, DFF]), t("ewd_f", [NE * DFF, D])]
        self.mod_b = t("mod_b", [1, 3072], F32)
        self.mod_f = t("mod_f", [8, 3072], F32)
        self.kT_b = t("kT_b", [NHEAD * 128, TOK])
        self.kT_f = t("kT_f", [NCORES * NHEAD * 128, TOK])
        self.v_b = t("v_b", [NHEAD * TOK, 128])
        self.v_f = t("v_f", [NCORES * NHEAD * TOK, 128])
        self.g_b = t("g_b", [8, TOK], F32)
        self.g_f = t("g_f", [NCORES * 8, TOK], F32)
        self.qT = t("qT", [NHEAD * 128, TOK])
        self.augK = t("augK", [6 * 8, S])
        self.augKo = t("augKo", [6 * 8, TOK])
        self.augQo = t("augQo", [6 * 8, TOK])
        self.o_d = t("o_d", [TOK, D], F32)
        self.hT_d = t("hT_d", [128, KC * TOK])
        self.done = False

    def dma(self, q, out, in_, reads=(), writes=()):
        return self.sc.add(q, lambda h, o=out, i=in_: h.dma_start(out=o, in_=i, allow_slow_non_contiguous=True), reads, writes, dma=True)

    def allgather(self, src, dst, reads, writes):
        def fn(h, s=src, d=dst):
            return h.collective_compute("AllGather", ALU.bypass, replica_groups=[list(range(NCORES))],
                                        ins=[s.ap().opt()], outs=[d.ap().opt()])
        return self.sc.add("pool", fn, reads, writes, cc=True)

    def consts(self):
        es = self.es
        self.cst = self.sb(es, "cst", [128, 512], F32)
        self.dma("sp", self.cst[:, :], self.cst_in[:, :], (), ("cst",))
        self.ident = self.cst[:, 0:128]
        self.cstb = self.sb(es, "cstb", [128, 256], BF16)
        self.sc.add("dve", lambda h: h.tensor_copy(out=self.cstb[:, :], in_=self.cst[:, 0:256]), ("cst",), ("cstb",))
        self.antiid_b = self.cstb[:, 128:256]
        self.ropec = self.cst[:, 384:512]
        self.onesb = self.sb(es, "onesb", [8, 2048], BF16)
        self.sc.add("pool", lambda h: h.memset(self.onesb[:, :], 1.0), (), ("onesb",))
        self.onesf = self.sb(es, "onesf", [8, 2048], F32)
        self.sc.add("pool", lambda h: h.memset(self.onesf[:, :], 1.0), (), ("onesf",))
        self.epsc = self.sb(es, "epsc", [128, 2], F32)
        self.sc.add("pool", lambda h: h.memset(self.epsc[:, 0:1], EPS), (), ("epsc",))
        self.sc.add("pool", lambda h: h.memset(self.epsc[:, 1:2], 1.0), (), ("epsc1",))

    def cast_to(self, src, dst, rows, key, piece=256):
        for r0 in range(0, rows, piece):
            r1 = min(rows, r0 + piece)
            self.dma("pool", dst[r0:r1, :], src[r0:r1, :], (), (key + str(r0),))
        return [key + str(r0) for r0 in range(0, rows, piece)]

    def weights_prep(self):
        def prep(src, bounce, full, rows, key, piece=256):
            ks = self.cast_to(src, bounce, rows, key + "_b", piece)
            self.allgather(bounce, full, ks, (key,))
        for l in range(2):
            prep(self.wfm_in[l], self.wfm_b[l], self.wfm_f[l], 256, "wfm%d" % l)
            prep(self.wv_in[l], self.wv_b[l], self.wv_f[l], 256, "wv%d" % l)
            prep(self.wo_in[l], self.wo_b[l], self.wo_f[l], 256, "wo%d" % l)
            if l == 0 and self.fwg_in is not None:
                prep(self.fwg_in, self.fw_b[0], self.fw_f[0], 256, "fw0")
                prep(self.fwu_in, self.fw_b[1], self.fw_f[1], 256, "fw1")
                prep(self.fwd_in, self.fw_b[2], self.fw_f[2], DFF // 8, "fw2", 448)
        self.prep_experts = lambda: [
            prep(self.ewg_in, self.ew_b[0], self.ew_f[0], D, "ew0", 128),
            prep(self.ewu_in, self.ew_b[1], self.ew_f[1], D, "ew1", 128),
            prep(self.ewd_in, self.ew_b[2], self.ew_f[2], DFF, "ew2", 512)]

    def mod_phase(self):
        nc, sc = self.nc, self.sc
        with self.scope() as es:
            cT = self.sb(es, "cT", [128, KC], F32)
            self.dma("sp", cT[:, :], self.c_in.ap().rearrange("(k p) o -> p (k o)", p=128), (), ("cT",))
            cond = self.sb(es, "cond", [128, KC], F32)
            sc.add("act", lambda h: h.activation(out=cond[:, :], in_=cT[:, :], func=AF.Silu), ("cT",), ("cond",))
            mrow = self.sb(es, "mrow", [1, 3072], F32)
            mbias = self.sb(es, "mbias", [1, 3072], F32)
            self.dma("sp", mbias[:, :], self.modb_in[:, :], (), ("mbias",))
            wb = [self.sb(es, "mw%d" % i, [128, KC, 512], F32) for i in range(2)]
            n = 0
            for l in range(2):
                for cc in range(3):
                    b = n % 2
                    self.dma("sp", wb[b][:, :, :],
                             self.modw_in[l].rearrange("(k p) n -> p k n", p=128)[:, :, cc * 512:(cc + 1) * 512],
                             (), ("mw%d" % b,))
                    pk = ("ps", n % 2)
                    for k in range(KC):
                        sc.add("pe", lambda h, k=k, b=b, n=n: h.matmul(
                            self.ps[n % 2][0:1, :], lhsT=cond[:, k:k + 1], rhs=wb[b][:, k, :],
                            start=(k == 0), stop=(k == KC - 1)), ("cond", "mw%d" % b), (pk,))
                    col = l * 1536 + cc * 512
                    sc.add("dve", lambda h, n=n, col=col: h.tensor_tensor(
                        out=mrow[:, col:col + 512], in0=self.ps[n % 2][0:1, :], in1=mbias[:, col:col + 512],
                        op=ALU.add), (pk, "mbias"), ("mrow%d" % n,))
                    n += 1
            self.dma("sp", self.mod_b[:, :], mrow[:, :], tuple("mrow%d" % i for i in range(6)), ("mod_b",))
            self.allgather(self.mod_b, self.mod_f, ("mod_b",), ("mod_f",))
        es = self.es
        self.modv = {}

    def mod_vec_src(self, l, c):
        return self.mod_f.ap().rearrange("r (l c i) -> l c r i", l=2, c=6)[l, c]

    def load_fm_vec(self, es, name, src_r_i, key_r, key_w):
        t = self.sb(es, name, [128, KC], F32)
        for r in range(8):
            self.dma("sp", t[:, 2 * r:2 * r + 2], src_r_i[r].rearrange("(j p) -> p j", p=128), key_r, (key_w,))
        return t

    def load_bc_vec(self, es, name, src_r_i, key_r, key_w):
        t = self.sb(es, name, [128, D], F32)
        self.dma("sp", t[:, :].rearrange("p (r i) -> p r i", r=8),
                 src_r_i.partition_broadcast(128), key_r, (key_w,))
        return t

    def normg_src(self, l, j):
        return self.normg_in[l * 4 + j:l * 4 + j + 1, :].rearrange("o (r i) -> (o r) i", r=8)

    def finish(self):
        sc = self.sc
        sc.barrier()
        sc.add("sp", lambda h: h.nop(), ("out",), ())

    def tap(self, name, src, key):
        shp = list(src.shape)
        rows = min(shp[0], max(128, (2 << 20) // (shp[1] * 4)))
        shp[0] = min(shp[0], 8 * rows)
        o = self.nc.dram_tensor("tap_" + name, shp, src.dtype, kind="ExternalOutput")
        for r0 in range(0, shp[0], rows):
            self.dma("sp", o[r0:r0 + rows, :], src[r0:r0 + rows, :], key, ("out",))

    def rms_rstd(self, es_bufs, src_ap, n, kr, tag, width):
        sc = self.sc
        junk, ss, sd, rstd = es_bufs
        sc.add("act", lambda h: h.activation(out=junk[:, 0:width], in_=src_ap, func=AF.Square, accum_out=ss[:, 0:1]),
               kr, (tag + "ss", tag + "junk"))
        sc.add("dve", lambda h: h.tensor_scalar(out=sd[:, 0:1], in0=ss[:, 0:1], scalar1=1.0 / n, scalar2=EPS,
                                                 op0=ALU.mult, op1=ALU.add), (tag + "ss",), (tag + "sd",))
        sc.add("act", lambda h: h.activation(out=sd[:, 1:2], in_=sd[:, 0:1], func=AF.Sqrt), (tag + "sd",), (tag + "sd2",))
        sc.add("dve", lambda h: h.reciprocal(out=rstd[:, 0:1], in_=sd[:, 1:2]), (tag + "sd2",), (tag + "rstd",))
        return tag + "rstd"

    def norm_to_hT(self, l, which, hT, xsrc_fn, router=None):
        sc = self.sc
        with self.scope() as es:
            j0 = 0 if which == 0 else 2
            c0 = 0 if which == 0 else 3
            gv = self.load_fm_vec(es, "gv", self.normg_src(l, j0), (), "gv")
            scv = self.load_fm_vec(es, "scv", self.mod_vec_src(l, c0 + 1), ("mod_f",), "scv")
            shv = self.load_fm_vec(es, "shv", self.mod_vec_src(l, c0), ("mod_f",), "shv")
            av = self.sb(es, "av", [128, KC], F32)
            sc.add("dve", lambda h: h.scalar_tensor_tensor(out=av[:, :], in0=scv[:, :], scalar=1.0, in1=gv[:, :],
                                                            op0=ALU.add, op1=ALU.mult), ("gv", "scv"), ("av",))
            xt = [self.sb(es, "xt%d" % i, [128, D], F32) for i in range(2)]
            xn = [self.sb(es, "xn%d" % i, [128, D], F32) for i in range(2)]
            junk = self.sb(es, "junk", [128, D], BF16)
            st = [[self.sb(es, "st%d_%d" % (i, j), [128, 2], F32) for j in range(3)] for i in range(2)]
            if router is not None:
                hf = [self.sb(es, "hf%d" % i, [128, 128], F32) for i in range(4)]
                hhi = [self.sb(es, "hhi%d" % i, [128, 128], BF16) for i in range(4)]
                hlo = [self.sb(es, "hlo%d" % i, [128, 128], BF16) for i in range(4)]
                rw = self.sb(es, "rw", [128, KC, 8], F32)
                self.dma("sp", rw[:, :, :], self.rw_in.ap().rearrange("(k p) e -> p k e", p=128), (), ("rw",))
                rhi = self.sb(es, "rhi", [128, KC, 128], BF16)
                rlo = self.sb(es, "rlo", [128, KC, 128], BF16)
                sc.add("pool", lambda h: h.memset(rhi[:, :, :], 0.0), (), ("rhi",))
                sc.add("pool", lambda h: h.memset(rlo[:, :, :], 0.0), (), ("rlo",))
                sc.add("dve", lambda h: h.tensor_copy(out=rhi[:, :, 0:8], in_=rw[:, :, :]), ("rw", "rhi"), ("rhi",))
                sc.add("dve", lambda h: h.tensor_tensor(out=rw[:, :, :], in0=rw[:, :, :], in1=rhi[:, :, 0:8], op=ALU.subtract),
                       ("rw", "rhi"), ("rw",))
                sc.add("dve", lambda h: h.tensor_copy(out=rlo[:, :, 0:8], in_=rw[:, :, :]), ("rw", "rlo"), ("rlo",))
                rbb = self.sb(es, "rbb", [128, 8], F32)
                self.dma("sp", rbb[:, :], self.rb_in[0:1, :].partition_broadcast(128), (), ("rbb",))
            nb = 0
            for tt in range(16):
                b = tt % 2
                src, kr = xsrc_fn(tt)
                self.dma("sp", xt[b][:, :], src, kr, ("xt%d" % b,))
                kk = self.rms_rstd((junk, st[b][0], st[b][1], st[b][2]), xt[b][:, :], D, ("xt%d" % b,), "n%d" % b, D)
                sc.add("dve", lambda h, b=b: h.tensor_scalar(out=xn[b][:, :], in0=xt[b][:, :], scalar1=st[b][2][:, 0:1],
                                                             scalar2=None, op0=ALU.mult), ("xt%d" % b, kk), ("xn%d" % b,))
                for q4 in range(4):
                    bank = 4 + (nb % 4) if router is not None else (nb % 8)
                    nb += 1
                    for j in range(4):
                        kc = q4 * 4 + j
                        sc.add("pe", lambda h, b=b, kc=kc, j=j, bank=bank: h.transpose(
                            out=self.ps[bank][:, j * 128:(j + 1) * 128], in_=xn[b][:, kc * 128:(kc + 1) * 128],
                            identity=self.ident), ("xn%d" % b, "cst"), (("ps", bank),))
                    for j in range(4):
                        kc = q4 * 4 + j
                        sc.add("act", lambda h, kc=kc, j=j, bank=bank, tt=tt: h.activation(
                            out=hT[:, kc, tt * 128:(tt + 1) * 128], in_=self.ps[bank][:, j * 128:(j + 1) * 128],
                            func=AF.Identity, scale=av[:, kc:kc + 1], bias=shv[:, kc:kc + 1]),
                            (("ps", bank), "av", "shv"), (("hT", tt),))
                    if router is not None and "rt" not in SKIP:
                        for j in range(4):
                            kc = q4 * 4 + j
                            hb = kc % 4
                            sc.add("act", lambda h, kc=kc, j=j, bank=bank, hb=hb: h.activation(
                                out=hf[hb][:, :], in_=self.ps[bank][:, j * 128:(j + 1) * 128], func=AF.Identity,
                                scale=av[:, kc:kc + 1], bias=shv[:, kc:kc + 1]),
                                (("ps", bank), "av", "shv"), ("hf%d" % hb,))
                            sc.add("dve", lambda h, hb=hb: h.tensor_copy(out=hhi[hb][:, :], in_=hf[hb][:, :]),
                                   ("hf%d" % hb,), ("hhi%d" % hb,))
                            sc.add("dve", lambda h, hb=hb: h.tensor_tensor(out=hf[hb][:, :], in0=hf[hb][:, :], in1=hhi[hb][:, :],
                                                                           op=ALU.subtract),
                                   ("hf%d" % hb, "hhi%d" % hb), ("hf%d" % hb,))
                            sc.add("dve", lambda h, hb=hb: h.tensor_copy(out=hlo[hb][:, :], in_=hf[hb][:, :]),
                                   ("hf%d" % hb,), ("hlo%d" % hb,))
                            sc.add("pe", lambda h, kc=kc, hb=hb, tt=tt: h.matmul(
                                self.ps[tt % 2][:, 0:128], lhsT=hhi[hb][:, :], rhs=rhi[:, kc, :],
                                start=(kc == 0), stop=False), ("hhi%d" % hb, "rhi"), (("ps", tt % 2),))
                            sc.add("pe", lambda h, kc=kc, hb=hb, tt=tt: h.matmul(
                                self.ps[tt % 2][:, 0:128], lhsT=hhi[hb][:, :], rhs=rlo[:, kc, :],
                                start=False, stop=False), ("hhi%d" % hb, "rlo"), (("ps", tt % 2),))
                            sc.add("pe", lambda h, kc=kc, hb=hb, tt=tt: h.matmul(
                                self.ps[tt % 2][:, 0:128], lhsT=hlo[hb][:, :], rhs=rhi[:, kc, :],
                                start=False, stop=(kc == KC - 1)), ("hlo%d" % hb, "rhi"), (("ps", tt % 2),))
                if router is not None and "lg" not in SKIP:
                    lg = router["lg"]
                    sc.add("dve", lambda h, tt=tt: h.tensor_tensor(out=lg[:, tt, :], in0=self.ps[tt % 2][:, 0:8],
                                                                   in1=rbb[:, :], op=ALU.add),
                           (("ps", tt % 2), "rbb"), (("lg", tt),))

    def rope_tables(self, es):
        sc = self.sc
        posi = self.sb(es, "posi", [128, TOK], I32)
        self.dma("sp", posi[:, :], self.pos_in[0:1, :].partition_broadcast(128), (), ("posi",))
        ang = self.sb(es, "ang", [128, TOK], F32)
        nr = self.sb(es, "nr", [128, TOK], F32)
        cosT = self.sb(es, "cosT", [128, TOK], F32)
        sinT = self.sb(es, "sinT", [128, TOK], F32)
        MAGIC = 12582912.0
        PI = math.pi
        LO = 2 * math.pi - 6.28125
        sc.add("dve", lambda h: h.tensor_copy(out=nr[:, :], in_=posi[:, :]), ("posi",), ("nr",))
        sc.add("dve", lambda h: h.tensor_scalar(out=ang[:, :], in0=nr[:, :], scalar1=self.ropec[:, 0:1], scalar2=None,
                                                 op0=ALU.mult), ("nr", "cst"), ("ang",))
        sc.add("dve", lambda h: h.tensor_scalar(out=nr[:, :], in0=ang[:, :], scalar1=1.0 / (2 * PI), scalar2=MAGIC,
                                                 op0=ALU.mult, op1=ALU.add), ("ang",), ("nr",))
        sc.add("dve", lambda h: h.tensor_scalar(out=nr[:, :], in0=nr[:, :], scalar1=MAGIC, scalar2=None,
                                                 op0=ALU.subtract), ("nr",), ("nr",))
        sc.add("dve", lambda h: h.scalar_tensor_tensor(out=ang[:, :], in0=nr[:, :], scalar=-6.28125, in1=ang[:, :],
                                                        op0=ALU.mult, op1=ALU.add), ("nr", "ang"), ("ang",))
        sc.add("dve", lambda h: h.scalar_tensor_tensor(out=ang[:, :], in0=nr[:, :], scalar=-LO, in1=ang[:, :],
                                                        op0=ALU.mult, op1=ALU.add), ("nr", "ang"), ("ang",))
        sc.add("dve", lambda h: h.tensor_scalar(out=ang[:, :], in0=ang[:, :], scalar1=PI, scalar2=-PI,
                                                 op0=ALU.min, op1=ALU.max), ("ang",), ("ang",))
        sc.add("act", lambda h: h.activation(out=sinT[:, :], in_=ang[:, :], func=AF.Sin), ("ang",), ("sinT",))
        sc.add("dve", lambda h: h.tensor_scalar(out=sinT[:, :], in0=sinT[:, :], scalar1=self.ropec[:, 1:2], scalar2=None,
                                                 op0=ALU.mult), ("sinT", "cst"), ("sinT",))
        sc.add("dve", lambda h: h.scalar_tensor_tensor(out=nr[:, :], in0=ang[:, :], scalar=-1.0, in1=ang[:, :],
                                                        op0=ALU.mult, op1=ALU.max), ("ang",), ("nr",))
        sc.add("dve", lambda h: h.tensor_scalar(out=nr[:, :], in0=nr[:, :], scalar1=-1.0, scalar2=PI / 2,
                                                 op0=ALU.mult, op1=ALU.add), ("nr",), ("nr",))
        sc.add("act", lambda h: h.activation(out=cosT[:, :], in_=nr[:, :], func=AF.Sin), ("nr",), ("cosT",))
        return cosT, sinT

    def proj_phase(self, l, hT):
        sc = self.sc
        hkeys = tuple(("hT", tt) for tt in range(16))
        with self.scope() as es:
            cosT, sinT = self.rope_tables(es)
            wt = [self.sb(es, "wt%d" % i, [128, KC, 512], BF16) for i in range(2)]
            stg = [self.sb(es, "stg%d" % i, [128, 512], BF16) for i in range(4)]
            rt = [self.sb(es, "rt%d" % i, [128, 512], F32) for i in range(2)]
            nstg = 0
            nbank = 0
            wsrc = self.wfm_f[l].ap().rearrange("(k p) n -> p k n", p=128)
            wgf = self.sb(es, "wgf", [128, KC, 8], F32)
            self.dma("sp", wgf[:, :, :], self.wgate_in[l].rearrange("(k p) e -> p k e", p=128), (), ("wgf",))
            wgb = self.sb(es, "wgb", [128, KC, 8], BF16)
            sc.add("dve", lambda h: h.tensor_copy(out=wgb[:, :, :], in_=wgf[:, :, :]), ("wgf",), ("wgb",))
            nb_f = self.sb(es, "nbf", [8, 1], F32)
            self.dma("sp", nb_f[:, :], self.bf_in[l:l + 1, :].rearrange("o e -> e o"), (), ("nbf",))
            sc.add("dve", lambda h: h.tensor_scalar(out=nb_f[:, :], in0=nb_f[:, :], scalar1=-1.0, scalar2=None,
                                                     op0=ALU.mult), ("nbf",), ("nbf",))
            gt = self.sb(es, "gt", [8, TOK], F32)
            for ts in range(4):
                bank = nbank % 8
                nbank += 1
                for k in range(KC):
                    sc.add("pe", lambda h, k=k, ts=ts, bank=bank: h.matmul(
                        self.ps[bank][0:8, :], lhsT=wgb[:, k, :], rhs=hT[:, k, ts * 512:(ts + 1) * 512],
                        start=(k == 0), stop=(k == KC - 1)), hkeys + ("wgb",), (("ps", bank),))
                sc.add("act", lambda h, ts=ts, bank=bank: h.activation(
                    out=gt[:, ts * 512:(ts + 1) * 512], in_=self.ps[bank][0:8, :], func=AF.Exp, scale=-1.0,
                    bias=nb_f[:, 0:1]), (("ps", bank), "nbf"), (("gt", ts),))
            sc.add("act", lambda h: h.activation(out=gt[:, :], in_=gt[:, :], func=AF.Ln, bias=self.epsc[0:8, 1:2]),
                   tuple(("gt", ts) for ts in range(4)) + ("epsc1",), ("gtl",))
            self.dma("sp", self.g_b[:, :], gt[:, :], ("gtl",), ("g_b",))
            self.allgather(self.g_b, self.g_f, ("g_b",), ("g_f",))
            for gi in range(10):
                wb = gi % 2
                self.dma("sp", wt[wb][:, :, :], wsrc[:, :, gi * 512:(gi + 1) * 512], ("wfm%d" % l,), ("wt%d" % wb,))
                for ts in range(4):
                    banks = []
                    for jj in range(4):
                        bank = nbank % 8
                        nbank += 1
                        banks.append(bank)
                        for k in range(KC):
                            sc.add("pe", lambda h, k=k, ts=ts, bank=bank, jj=jj, wb=wb: h.matmul(
                                self.ps[bank][:, :], lhsT=wt[wb][:, k, jj * 128:(jj + 1) * 128],
                                rhs=hT[:, k, ts * 512:(ts + 1) * 512], start=(k == 0), stop=(k == KC - 1)),
                                hkeys + ("wt%d" % wb,), (("ps", bank),))
                    tsl = slice(ts * 512, (ts + 1) * 512)
                    if gi < 6:
                        for jj in range(4):
                            hh = gi * 2 + jj // 2
                            dst = (self.qT if jj % 2 == 0 else self.kT_b)
                            s_ = nstg % 4
                            nstg += 1
                            eng = "act" if jj % 2 == 0 else "dve"
                            if eng == "act":
                                sc.add("act", lambda h, s_=s_, bank=banks[jj]: h.activation(
                                    out=stg[s_][:, :], in_=self.ps[bank][:, :], func=AF.Copy),
                                    (("ps", banks[jj]),), ("stg%d" % s_,))
                            else:
                                sc.add("dve", lambda h, s_=s_, bank=banks[jj]: h.tensor_copy(
                                    out=stg[s_][:, :], in_=self.ps[bank][:, :]), (("ps", banks[jj]),), ("stg%d" % s_,))
                            self.dma("sp", dst[hh * 128:(hh + 1) * 128, tsl], stg[s_][:, :], ("stg%d" % s_,),
                                     ("qT" if jj % 2 == 0 else "kT_b",))
                    else:
                        hh = 12 + (gi - 6)
                        for pr in range(2):
                            b0, b1 = banks[2 * pr], banks[2 * pr + 1]
                            s_ = nstg % 4
                            nstg += 1
                            sc.add("dve", lambda h, b0=b0, ts=ts: h.tensor_tensor(
                                out=rt[0][:, :], in0=self.ps[b0][:, :], in1=cosT[:, ts * 512:(ts + 1) * 512], op=ALU.mult),
                                (("ps", b0), "cosT"), ("rt0",))
                            sc.add("dve", lambda h, b1=b1, ts=ts: h.tensor_tensor(
                                out=rt[1][:, :], in0=self.ps[b1][:, :], in1=sinT[:, ts * 512:(ts + 1) * 512], op=ALU.mult),
                                (("ps", b1), "sinT"), ("rt1",))
                            sc.add("pool", lambda h, s_=s_: h.tensor_tensor(
                                out=stg[s_][:, :], in0=rt[0][:, :], in1=rt[1][:, :], op=ALU.add),
                                ("rt0", "rt1"), ("stg%d" % s_,))
                            dst = self.qT if pr == 0 else self.kT_b
                            self.dma("sp", dst[hh * 128:(hh + 1) * 128, tsl], stg[s_][:, :], ("stg%d" % s_,),
                                     ("qT" if pr == 0 else "kT_b",))
            self.allgather(self.kT_b, self.kT_f, ("kT_b",), ("kT_f",))
            vsrc = self.wv_f[l].ap().rearrange("(k p) n -> p k n", p=128)
            vdst = self.v_b.ap().rearrange("(h t) d -> t h d", h=NHEAD)
            for vc in range(4):
                wb = vc % 2
                self.dma("sp", wt[wb][:, :, :], vsrc[:, :, vc * 512:(vc + 1) * 512], ("wv%d" % l,), ("wt%d" % wb,))
                for tt in range(16):
                    bank = nbank % 8
                    nbank += 1
                    for k in range(KC):
                        sc.add("pe", lambda h, k=k, tt=tt, bank=bank, wb=wb: h.matmul(
                            self.ps[bank][:, :], lhsT=hT[:, k, tt * 128:(tt + 1) * 128], rhs=wt[wb][:, k, :],
                            start=(k == 0), stop=(k == KC - 1)), hkeys + ("wt%d" % wb,), (("ps", bank),))
                    s_ = nstg % 4
                    nstg += 1
                    if tt % 2 == 0:
                        sc.add("act", lambda h, s_=s_, bank=bank: h.activation(
                            out=stg[s_][:, :], in_=self.ps[bank][:, :], func=AF.Copy), (("ps", bank),), ("stg%d" % s_,))
                    else:
                        sc.add("dve", lambda h, s_=s_, bank=bank: h.tensor_copy(
                            out=stg[s_][:, :], in_=self.ps[bank][:, :]), (("ps", bank),), ("stg%d" % s_,))
                    self.dma("sp", vdst[tt * 128:(tt + 1) * 128, vc * 4:(vc + 1) * 4, :],
                             stg[s_][:, :].rearrange("p (h d) -> p h d", h=4), ("stg%d" % s_,), ("v_b",))
            self.allgather(self.v_b, self.v_f, ("v_b",), ("v_f",))

    def f_phase(self, l):
        sc = self.sc
        SQ = math.sqrt(128.0)
        with self.scope() as es:
            Gall = self.sb(es, "Gall", [8, 8, 2048], F32)
            self.dma("sp", Gall[:, :, :], self.g_f.ap().rearrange("(r h) t -> h r t", r=8), ("g_f",), ("Gall",))
            Gown = self.sb(es, "Gown", [8, 2048], F32)
            self.dma("sp", Gown[:, :], self.g_b[:, :], ("g_b",), ("Gown",))
            cum = self.sb(es, "cum", [8, 8, 2048], F32)
            cown = self.sb(es, "cown", [8, 2048], F32)
            cex = self.sb(es, "cex", [8, 32], F32)
            selb = self.sb(es, "selb", [8, 128], F32)
            self.dma("sp", selb[:, :], self.sel_in[:, :], (), ("selb",))
            pown = self.sb(es, "pown", [8, 4], F32)
            tmp = self.sb(es, "ftmp", [8, 32], F32)
            sc.add("pool", lambda h: h.memset(cex[:, :], 0.0), (), ("cex",))
            for u in range(32):
                rk, sl = tile_loc(u)
                init = 0.0 if u == 0 else cex[:, u:u + 1]
                sc.add("dve", lambda h, rk=rk, sl=sl, init=init: h.tensor_tensor_scan(
                    out=cum[:, rk, sl * 512:(sl + 1) * 512], data0=self.onesf[:, 0:512],
                    data1=Gall[:, rk, sl * 512:(sl + 1) * 512], initial=init, op0=ALU.mult, op1=ALU.add),
                    ("Gall", "onesf", "cex"), (("cum", rk),))
                if u < 31:
                    sc.add("dve", lambda h, rk=rk, sl=sl, u=u: h.tensor_copy(
                        out=cex[:, u + 1:u + 2], in_=cum[:, rk, sl * 512 + 511:sl * 512 + 512]),
                        (("cum", rk),), ("cex",))
            for s in range(4):
                sc.add("dve", lambda h, s=s: h.tensor_tensor(out=tmp[:, :], in0=cex[:, :], in1=selb[:, s * 32:(s + 1) * 32],
                                                             op=ALU.mult), ("cex", "selb"), ("ftmp",))
                sc.add("dve", lambda h, s=s: h.reduce_sum(out=pown[:, s:s + 1], in_=tmp[:, :], axis=AX.X),
                       ("ftmp",), ("pown",))
                sc.add("dve", lambda h, s=s: h.tensor_tensor_scan(
                    out=cown[:, s * 512:(s + 1) * 512], data0=self.onesf[:, 0:512], data1=Gown[:, s * 512:(s + 1) * 512],
                    initial=pown[:, s:s + 1], op0=ALU.mult, op1=ALU.add), ("Gown", "onesf", "pown"), ("cown",))
            hb = [self.sb(es, "hb%d" % i, [8, 2048], BF16) for i in range(4)]
            nhb = [0]
            aK = self.augK.ap().rearrange("(h r) (k t) -> h r k t", r=6, k=8)
            aKo = self.augKo.ap().rearrange("(h r) t -> h r t", r=6)
            aQo = self.augQo.ap().rearrange("(h r) t -> h r t", r=6)

            def split_store(src, kr, kname, dstK, dstQ):
                sc.add("dve", lambda h: h.tensor_scalar(out=src, in0=src, scalar1=SQ, scalar2=None, op0=ALU.mult),
                       (kr,), (kr,))
                for i in range(3):
                    b = nhb[0] % 4
                    nhb[0] += 1
                    sc.add("dve", lambda h, b=b: h.tensor_copy(out=hb[b][:, :], in_=src), (kr,), ("hb%d" % b,))
                    self.dma("sp", dstK(3 + i), hb[b][:, :], ("hb%d" % b,), (kname,))
                    if i < 2:
                        sc.add("dve", lambda h, b=b: h.tensor_tensor(out=src, in0=src, in1=hb[b][:, :], op=ALU.subtract),
                               (kr, "hb%d" % b), (kr,))
                    if dstQ is not None:
                        b2 = nhb[0] % 4
                        nhb[0] += 1
                        sc.add("dve", lambda h, b=b, b2=b2: h.tensor_scalar(out=hb[b2][:, :], in0=hb[b][:, :], scalar1=-1.0,
                                                                            scalar2=None, op0=ALU.mult),
                               ("hb%d" % b,), ("hb%d" % b2,))
                        self.dma("sp", dstQ(i), hb[b2][:, :], ("hb%d" % b2,), ("augQo",))
            for rk in range(8):
                split_store(cum[:, rk, :], ("cum", rk), "augK", lambda row, rk=rk: aK[:, row, rk, :], None)
            split_store(cown[:, :], "cown", "augKo", lambda row: aKo[:, row, :], lambda row: aQo[:, row, :])
            if l == 0:
                for row in range(3):
                    for rk in range(8):
                        self.dma("sp", aK[:, row, rk, :], self.onesb[:, :], ("onesb",), ("augK",))
                    self.dma("sp", aKo[:, row, :], self.onesb[:, :], ("onesb",), ("augKo",))
                    self.dma("sp", aQo[:, 3 + row, :], self.onesb[:, :], ("onesb",), ("augQo",))

    def attn_phase(self, l):
        sc = self.sc
        SQ = math.sqrt(128.0)
        NEG = 3.0e5
        lam_init = 0.8 - 0.6 * math.exp(-0.3 * l)
        with self.scope() as es:
            tb = self.sb(es, "tb", [128, 256], F32)
            self.dma("sp", tb[:, :], self.tb_in[:, :], (), ("tb",))
            negC = self.sb(es, "negC", [128, 8, 512], BF16)
            negB = self.sb(es, "negB", [128, 8, 512], BF16)
            _ms = self.scope()
            _ms.__enter__()
            stage = self.sb(es, "mstage", [128, 8, 512], F32)
            self.dma("sp", stage[:, :, :], self.cmask_in.ap().rearrange("p (j q) -> p j q", j=8), (), ("mstage",))
            sc.add("dve", lambda h: h.tensor_scalar(out=negC[:, :, :], in0=stage[:, :, :], scalar1=-1.0, scalar2=NEG,
                                                     op0=ALU.add, op1=ALU.mult), ("mstage",), ("negC",))
            self.dma("sp", stage[:, :, :], self.bmask_in.ap().rearrange("p (j q) -> p j q", j=8), (), ("mstage",))
            sc.add("dve", lambda h: h.tensor_scalar(out=negB[:, :, :], in0=stage[:, :, :], scalar1=-1.0, scalar2=NEG,
                                                     op0=ALU.add, op1=ALU.mult), ("mstage",), ("negB",))
            _ms.__exit__()
            identb = self.cstb[:, 0:128]
            onw = self.sb(es, "onw", [128, 3, 128], F32)
            self.dma("sp", onw[:, :, :], self.onorm_in[l].partition_broadcast(128), (), ("onw",))
            sc.add("dve", lambda h: h.tensor_scalar(out=onw[:, 2, :], in0=onw[:, 2, :], scalar1=1.0 - lam_init,
                                                     scalar2=None, op0=ALU.mult), ("onw",), ("onw",))
            lamb = self.sb(es, "lamb", [128, 256], F32)
            self.dma("sp", lamb[:, :], self.lam_in[l:l + 1, :].partition_broadcast(128), (), ("lamb",))
            lt = self.sb(es, "lt", [128, 128], F32)
            ls = self.sb(es, "ls", [128, 4], F32)
            for i in range(2):
                sc.add("dve", lambda h, i=i: h.tensor_tensor(out=lt[:, i * 64:(i + 1) * 64], in0=lamb[:, i * 128:i * 128 + 64],
                                                             in1=lamb[:, i * 128 + 64:i * 128 + 128], op=ALU.mult),
                       ("lamb",), ("lt",))
                sc.add("dve", lambda h, i=i: h.reduce_sum(out=ls[:, i:i + 1], in_=lt[:, i * 64:(i + 1) * 64], axis=AX.X),
                       ("lt",), ("ls",))
            sc.add("act", lambda h: h.activation(out=ls[:, 2:4], in_=ls[:, 0:2], func=AF.Exp), ("ls",), ("ls2",))
            nlam = self.sb(es, "nlam", [128, 1], F32)
            sc.add("dve", lambda h: h.scalar_tensor_tensor(out=nlam[:, 0:1], in0=ls[:, 3:4], scalar=-lam_init, in1=ls[:, 2:3],
                                                            op0=ALU.add, op1=ALU.subtract), ("ls2",), ("nlam",))
            kt = self.sb(es, "kt", [128, 8, 2048], BF16)
            kto = self.sb(es, "kto", [128, 2048], BF16)
            vt = self.sb(es, "vt", [128, 128, 129], BF16)
            vto = self.sb(es, "vto", [128, 16, 129], BF16)
            sc.add("pool", lambda h: h.memset(vt[:, :, 128:129], 1.0), (), ("vt1",))
            sc.add("pool", lambda h: h.memset(vto[:, :, 128:129], 1.0), (), ("vto1",))
            qt = self.sb(es, "qt", [128, 2048], BF16)
            pT = [self.sb(es, "pT%d" % i, [128, 512], BF16) for i in range(6)]
            fin = [self.sb(es, "fin%d" % i, [128, 8], F32) for i in range(2)]
            onb = [self.sb(es, "onb%d" % i, [128, 128], F32) for i in range(2)]
            ofb = [self.sb(es, "ofb%d" % i, [128, 128], F32) for i in range(2)]
            junk = self.sb(es, "ajunk", [128, 128], BF16)
            kTf = self.kT_f.ap().rearrange("(r h d) t -> d r h t", r=8, h=NHEAD)
            vf = self.v_f.ap().rearrange("(r h k p) d -> p r h k d", r=8, h=NHEAD, k=16)
            vb_ = self.v_b.ap().rearrange("(h k p) d -> p h k d", h=NHEAD, k=16)
            aK = self.augK.ap().rearrange("(h r) (k t) -> h r k t", r=6, k=8)
            aKo = self.augKo.ap().rearrange("(h r) t -> h r t", r=6)
            aQo = self.augQo.ap().rearrange("(h r) t -> h r t", r=6)
            npt = 0
            nsb = 0
            nfin = 0
            MAXU = [7, 15, 23, 31]
            BC = [list(range(0, 7)), list(range(7, 15)), list(range(15, 23)), list(range(23, 31))]
            gsc = None
            for hh in range(NHEAD):
                typ = "A" if hh < 6 else ("B" if hh < 12 else "C")
                scale = 64.0 ** -0.5 if typ == "C" else 128.0 ** -0.5
                if hh in (0, 6, 12):
                    if gsc is not None:
                        gsc.__exit__()
                        gsc = None
                    if hh == 0:
                        gsc = self.scope()
                        gsc.__enter__()
                        agk = self.sb(es, "agk", [8, 8, 2048], BF16)
                        agko = self.sb(es, "agko", [8, 2048], BF16)
                        agqo = self.sb(es, "agqo", [8, 2048], BF16)
                    elif hh == 6:
                        gsc = self.scope()
                        gsc.__enter__()
                        bstage = self.sb(es, "bstage", [128, 4, 512], F32)
                        biasb = self.sb(es, "biasb", [128, 8, 512], BF16)
                self.dma("sp", kt[:, :, :], kTf[:, :, hh, :], ("kT_f",), ("kt",))
                self.dma("sp", kto[:, :], self.kT_b[hh * 128:(hh + 1) * 128, :], ("kT_b",), ("kto",))
                for rk in range(8):
                    self.dma("sp", vt[:, rk * 16:(rk + 1) * 16, 0:128], vf[:, rk, hh, :, :], ("v_f",), ("vt",))
                self.dma("sp", vto[:, :, 0:128], vb_[:, hh, :, :], ("v_b",), ("vto",))
                self.dma("sp", qt[:, :], self.qT[hh * 128:(hh + 1) * 128, :], ("qT",), ("qt",))
                if typ == "A":
                    self.dma("sp", agk[0:6, :, :], aK[hh], ("augK",), ("agk",))
                    self.dma("sp", agko[0:6, :], aKo[hh], ("augKo",), ("agko",))
                    self.dma("sp", agqo[0:6, :], aQo[hh], ("augQo",), ("agqo",))
                if typ == "B":
                    hbh = hh - 6
                    for half in range(2):
                        for j4 in range(4):
                            jj = half * 4 + j4
                            srcap = bass.AP(tensor=self.relE_in, offset=(l * 6 + hbh) * ELEN + 896 - 128 * jj,
                                            ap=[[1, 128], [1, 512]])
                            self.dma("sp", bstage[:, j4, :], srcap, (), ("bstage",))
                        sc.add("dve", lambda h, half=half: h.scalar_tensor_tensor(
                            out=biasb[:, half * 4:(half + 1) * 4, :], in0=bstage[:, :, :], scalar=SQ,
                            in1=negB[:, half * 4:(half + 1) * 4, :], op0=ALU.mult, op1=ALU.add),
                            ("bstage", "negB"), ("biasb",))
                for s in range(4):
                    qsl = slice(s * 512, (s + 1) * 512)
                    tiles = []
                    cands = BC[s] if typ == "B" else list(range(MAXU[s]))
                    for ci, u in enumerate(cands):
                        rk, sl = tile_loc(u)
                        col = (128 + s * 8 + ci) if typ == "B" else (s * 32 + u)
                        for jq in range(4):
                            tiles.append((False, rk, sl * 4 + jq, col, jq))
                    for jq in range(4):
                        tiles.append((True, None, s * 4 + jq, 255, jq))
                    nt = len(tiles)
                    ob = 4 if (nfin % 2 == 0 or typ == "C") else 6
                    for ti, (diag, rk, kidx, col, jq) in enumerate(tiles):
                        if diag:
                            kap = lambda lo, hi_, kidx=kidx: kto[lo:hi_, kidx * 128:(kidx + 1) * 128]
                            vap = vto[:, kidx, :]
                            kkeys, vkey = ("kto",), ("vto", "vto1")
                        else:
                            kap = lambda lo, hi_, rk=rk, kidx=kidx: kt[lo:hi_, rk, kidx * 128:(kidx + 1) * 128]
                            vap = vt[:, rk * 16 + kidx, :]
                            kkeys, vkey = ("kt",), ("vt", "vt1")
                        nmap = 2 if typ == "C" else 1
                        sbanks = []
                        for m in range(nmap):
                            bank = nsb % 4
                            nsb += 1
                            sbanks.append(bank)
                            lo, hi_ = (m * 64, m * 64 + 64) if typ == "C" else (0, 128)
                            single = (typ == "C" and not diag)
                            sc.add("pe", lambda h, bank=bank, kap=kap, lo=lo, hi_=hi_, single=single, qsl=qsl: h.matmul(
                                self.ps[bank][:, :], lhsT=kap(lo, hi_), rhs=qt[lo:hi_, qsl], start=True, stop=single),
                                kkeys + ("qt",), (("ps", bank),))
                            if typ == "A":
                                if diag:
                                    sc.add("pe", lambda h, bank=bank, kidx=kidx, qsl=qsl: h.matmul(
                                        self.ps[bank][:, :], lhsT=agko[0:6, kidx * 128:(kidx + 1) * 128], rhs=agqo[0:6, qsl],
                                        start=False, stop=False), ("agko", "agqo"), (("ps", bank),))
                                    sc.add("pe", lambda h, bank=bank, jq=jq: h.matmul(
                                        self.ps[bank][:, :], lhsT=identb, rhs=negC[:, jq, :], start=False, stop=True),
                                        ("cstb", "negC"), (("ps", bank),))
                                else:
                                    sc.add("pe", lambda h, bank=bank, rk=rk, kidx=kidx, qsl=qsl: h.matmul(
                                        self.ps[bank][:, :], lhsT=agk[0:6, rk, kidx * 128:(kidx + 1) * 128], rhs=agqo[0:6, qsl],
                                        start=False, stop=True), ("agk", "agqo"), (("ps", bank),))
                            elif typ == "B":
                                jj = (4 + jq) if diag else jq
                                sc.add("pe", lambda h, bank=bank, jj=jj: h.matmul(
                                    self.ps[bank][:, :], lhsT=self.antiid_b, rhs=biasb[:, jj, :], start=False, stop=True),
                                    ("cstb", "biasb"), (("ps", bank),))
                            elif diag:
                                sc.add("pe", lambda h, bank=bank, jq=jq: h.matmul(
                                    self.ps[bank][:, :], lhsT=identb, rhs=negC[:, 4 + jq, :], start=False, stop=True),
                                    ("cstb", "negC"), (("ps", bank),))
                        pbs = []
                        for m in range(nmap):
                            pb = npt % 6
                            npt += 1
                            pbs.append(pb)
                            sc.add("act", lambda h, pb=pb, bank=sbanks[m], col=col, scale=scale: h.activation(
                                out=pT[pb][:, :], in_=self.ps[bank][:, :], func=AF.Exp, scale=scale,
                                bias=tb[:, col:col + 1]), (("ps", sbanks[m]), "tb"), ("pT%d" % pb,))
                        for m in range(nmap):
                            for qs in range(4):
                                if typ == "C":
                                    obank, ocol = 4 + qs, m * 129
                                else:
                                    obank, ocol = ob + qs // 2, (qs % 2) * 129
                                sc.add("pe", lambda h, pb=pbs[m], qs=qs, obank=obank, ocol=ocol, vap=vap, ti=ti, nt=nt: h.matmul(
                                    self.ps[obank][:, ocol:ocol + 129], lhsT=pT[pb][:, qs * 128:(qs + 1) * 128], rhs=vap,
                                    start=(ti == 0), stop=(ti == nt - 1)), ("pT%d" % pbs[m],) + vkey, (("ps", obank),))
                    grp = 0 if typ == "A" else (1 if typ == "B" else 2)
                    for qs in range(4):
                        fb = nfin % 2
                        if typ == "C":
                            obank, ocol = 4 + qs, 0
                        else:
                            obank, ocol = ob + qs // 2, (qs % 2) * 129
                        f = fin[fb]
                        okey = (("ps", obank),)
                        O0 = self.ps[obank][:, ocol:ocol + 128]
                        sc.add("dve", lambda h, f=f, obank=obank, ocol=ocol: h.reciprocal(
                            out=f[:, 0:1], in_=self.ps[obank][:, ocol + 128:ocol + 129]), okey, ("fin%d" % fb,))
                        sc.add("dve", lambda h, f=f, O0=O0, fb=fb: h.tensor_scalar(
                            out=onb[fb][:, :], in0=O0, scalar1=f[:, 0:1], scalar2=None, op0=ALU.mult),
                            okey + ("fin%d" % fb,), ("onb%d" % fb,))
                        if typ == "C":
                            sc.add("dve", lambda h, f=f, obank=obank: h.reciprocal(
                                out=f[:, 1:2], in_=self.ps[obank][:, 129 + 128:129 + 129]), okey, ("finb%d" % fb,))
                            sc.add("dve", lambda h, f=f: h.tensor_scalar(out=f[:, 1:2], in0=f[:, 1:2], scalar1=nlam[:, 0:1],
                                                                         scalar2=None, op0=ALU.mult),
                                   ("finb%d" % fb, "nlam"), ("finb%d" % fb,))
                            sc.add("dve", lambda h, f=f, obank=obank, fb=fb: h.scalar_tensor_tensor(
                                out=onb[fb][:, :], in0=self.ps[obank][:, 129:129 + 128], scalar=f[:, 1:2], in1=onb[fb][:, :],
                                op0=ALU.mult, op1=ALU.add), okey + ("finb%d" % fb, "onb%d" % fb), ("onb%d" % fb,))
                        sc.add("act", lambda h, f=f, fb=fb: h.activation(out=junk[:, :], in_=onb[fb][:, :], func=AF.Square,
                                                                         accum_out=f[:, 2:3]),
                               ("onb%d" % fb,), ("finc%d" % fb, "ajunk"))
                        sc.add("dve", lambda h, f=f: h.tensor_scalar(out=f[:, 3:4], in0=f[:, 2:3], scalar1=1.0 / 128, scalar2=EPS,
                                                                     op0=ALU.mult, op1=ALU.add), ("finc%d" % fb,), ("find%d" % fb,))
                        sc.add("act", lambda h, f=f: h.activation(out=f[:, 4:5], in_=f[:, 3:4], func=AF.Sqrt),
                               ("find%d" % fb,), ("fine%d" % fb,))
                        sc.add("dve", lambda h, f=f: h.reciprocal(out=f[:, 5:6], in_=f[:, 4:5]), ("fine%d" % fb,), ("finf%d" % fb,))
                        sc.add("dve", lambda h, f=f, fb=fb, grp=grp: h.scalar_tensor_tensor(
                            out=ofb[fb][:, :], in0=onb[fb][:, :], scalar=f[:, 5:6], in1=onw[:, grp, :], op0=ALU.mult, op1=ALU.mult),
                            ("onb%d" % fb, "finf%d" % fb, "onw"), ("ofb%d" % fb,))
                        self.dma("sp", self.o_d[s * 512 + qs * 128:s * 512 + (qs + 1) * 128, hh * 128:(hh + 1) * 128],
                                 ofb[fb][:, :], ("ofb%d" % fb,), ("o_d",))
                        nfin += 1

    def bc_prod(self, es, name, l, cmod, jnorm):
        sc = self.sc
        a = self.load_bc_vec(es, name + "a", self.mod_vec_src(l, cmod), ("mod_f",), name + "a")
        b = self.load_bc_vec(es, name + "b", self.normg_src(l, jnorm), (), name + "b")
        sc.add("pool", lambda h: h.tensor_tensor(out=a[:, :], in0=a[:, :], in1=b[:, :], op=ALU.mult),
               (name + "a", name + "b"), (name,))
        return a

    def o_phase(self, l):
        sc = self.sc
        xsrc = self.x_in if l == 0 else self.out
        with self.scope() as es:
            wo = self.sb(es, "wo", [128, KC, D], BF16)
            self.dma("sp", wo[:, :, :], self.wo_f[l].ap().rearrange("(k p) n -> p k n", p=128), ("wo%d" % l,), ("wo",))
            gm1 = self.bc_prod(es, "gm1", l, 2, 1)
            ot = [self.sb(es, "ot%d" % i, [128, D], F32) for i in range(2)]
            xt = [self.sb(es, "oxt%d" % i, [128, D], F32) for i in range(2)]
            tmp = [self.sb(es, "otmp%d" % i, [128, D], F32) for i in range(2)]
            oT = [self.sb(es, "oT%d" % i, [128, KC, 128], BF16) for i in range(2)]
            st = [self.sb(es, "ost%d" % i, [128, 8], F32) for i in range(2)]
            junk = self.sb(es, "ojunk", [128, 512], BF16)
            for tt in range(16):
                b = tt % 2
                rows = slice(tt * 128, (tt + 1) * 128)
                self.dma("sp", ot[b][:, :], self.o_d[rows, :], ("o_d",), ("ot%d" % b,))
                self.dma("sp", xt[b][:, :], xsrc[rows, :], (("out", tt),) if l else (), ("oxt%d" % b,))
                for q4 in range(4):
                    bank = q4
                    for j_ in range(4):
                        kc = q4 * 4 + j_
                        sc.add("pe", lambda h, b=b, kc=kc, j_=j_, bank=bank: h.transpose(
                            out=self.ps[bank][:, j_ * 128:(j_ + 1) * 128], in_=ot[b][:, kc * 128:(kc + 1) * 128],
                            identity=self.ident), ("ot%d" % b, "cst"), (("ps", bank),))
                    if q4 % 2 == 0:
                        sc.add("act", lambda h, b=b, q4=q4, bank=bank: h.activation(
                            out=oT[b][:, q4 * 4:(q4 + 1) * 4, :], in_=self.ps[bank][:, :].rearrange("p (a c) -> p a c", a=4),
                            func=AF.Copy), (("ps", bank),), (("oT", b, q4),))
                    else:
                        sc.add("dve", lambda h, b=b, q4=q4, bank=bank: h.tensor_copy(
                            out=oT[b][:, q4 * 4:(q4 + 1) * 4, :], in_=self.ps[bank][:, :].rearrange("p (a c) -> p a c", a=4)),
                            (("ps", bank),), (("oT", b, q4),))
                okeys = tuple(("oT", b, q4) for q4 in range(4))
                for dq in range(4):
                    bank = 4 + dq
                    for kc in range(KC):
                        sc.add("pe", lambda h, b=b, kc=kc, dq=dq, bank=bank: h.matmul(
                            self.ps[bank][:, :], lhsT=oT[b][:, kc, :], rhs=wo[:, kc, dq * 512:(dq + 1) * 512],
                            start=(kc == 0), stop=(kc == KC - 1)), okeys + ("wo",), (("ps", bank),))
                    sc.add("act", lambda h, b=b, dq=dq, bank=bank: h.activation(
                        out=junk[:, :], in_=self.ps[bank][:, :], func=AF.Square, accum_out=st[b][:, dq:dq + 1]),
                        (("ps", bank),), (("ost", b, dq), "ojunk"))
                sc.add("dve", lambda h, b=b: h.reduce_sum(out=st[b][:, 4:5], in_=st[b][:, 0:4], axis=AX.X),
                       tuple(("ost", b, dq) for dq in range(4)), (("ost4", b),))
                sc.add("dve", lambda h, b=b: h.tensor_scalar(out=st[b][:, 5:6], in0=st[b][:, 4:5], scalar1=1.0 / D, scalar2=EPS,
                                                             op0=ALU.mult, op1=ALU.add), (("ost4", b),), (("ost5", b),))
                sc.add("act", lambda h, b=b: h.activation(out=st[b][:, 6:7], in_=st[b][:, 5:6], func=AF.Sqrt),
                       (("ost5", b),), (("ost6", b),))
                sc.add("dve", lambda h, b=b: h.reciprocal(out=st[b][:, 7:8], in_=st[b][:, 6:7]), (("ost6", b),), (("ost7", b),))
                for dq in range(4):
                    bank = 4 + dq
                    cs = slice(dq * 512, (dq + 1) * 512)
                    sc.add("dve", lambda h, b=b, bank=bank, cs=cs: h.scalar_tensor_tensor(
                        out=tmp[b][:, cs], in0=self.ps[bank][:, :], scalar=st[b][:, 7:8], in1=gm1[:, cs],
                        op0=ALU.mult, op1=ALU.mult), (("ps", bank), ("ost7", b), "gm1"), (("otmp", b, dq),))
                    sc.add("pool", lambda h, b=b, cs=cs: h.tensor_tensor(out=xt[b][:, cs], in0=tmp[b][:, cs], in1=xt[b][:, cs],
                                                                         op=ALU.add),
                           (("otmp", b, dq), "oxt%d" % b), ("oxt%d" % b,))
                self.dma("sp", self.out[rows, :], xt[b][:, :], ("oxt%d" % b,), (("out", tt),))

    def gating(self, lg, comb):
        sc = self.sc
        with self.scope() as es:
            m = [self.sb(es, "gm%d" % i, [128, 8], F32) for i in range(2)]
            k1 = [self.sb(es, "gk1%d" % i, [128, 8], F32) for i in range(2)]
            k2 = [self.sb(es, "gk2%d" % i, [128, 8], F32) for i in range(2)]
            l2 = [self.sb(es, "gl2%d" % i, [128, 8], F32) for i in range(2)]
            for tt in range(16):
                b = tt % 2
                M, K1, K2, L2 = m[b], k1[b], k2[b], l2[b]
                kb = "g%d" % b
                sc.add("dve", lambda h, M=M, tt=tt: h.reduce_max(out=M[:, 0:1], in_=lg[:, tt, :], axis=AX.X),
                       (("lg", tt),), (kb + "m1",))
                sc.add("dve", lambda h, M=M, K1=K1, tt=tt: h.tensor_scalar(out=K1[:, :], in0=lg[:, tt, :], scalar1=M[:, 0:1],
                                                                           scalar2=None, op0=ALU.is_equal),
                       (("lg", tt), kb + "m1"), (kb + "k1",))
                sc.add("dve", lambda h, K1=K1, L2=L2, tt=tt: h.scalar_tensor_tensor(
                    out=L2[:, :], in0=K1[:, :], scalar=-1.0e30, in1=lg[:, tt, :], op0=ALU.mult, op1=ALU.add),
                    (kb + "k1", ("lg", tt)), (kb + "l2",))
                sc.add("dve", lambda h, M=M, L2=L2: h.reduce_max(out=M[:, 1:2], in_=L2[:, :], axis=AX.X),
                       (kb + "l2",), (kb + "m2",))
                sc.add("dve", lambda h, M=M, K2=K2, L2=L2: h.tensor_scalar(out=K2[:, :], in0=L2[:, :], scalar1=M[:, 1:2],
                                                                           scalar2=None, op0=ALU.is_equal),
                       (kb + "l2", kb + "m2"), (kb + "k2",))
                sc.add("dve", lambda h, M=M: h.tensor_tensor(out=M[:, 2:3], in0=M[:, 1:2], in1=M[:, 0:1], op=ALU.subtract),
                       (kb + "m1", kb + "m2"), (kb + "d",))
                sc.add("act", lambda h, M=M: h.activation(out=M[:, 3:4], in_=M[:, 2:3], func=AF.Exp), (kb + "d",), (kb + "e",))
                sc.add("dve", lambda h, M=M: h.tensor_scalar(out=M[:, 4:5], in0=M[:, 3:4], scalar1=1.0, scalar2=None,
                                                             op0=ALU.add), (kb + "e",), (kb + "den",))
                sc.add("dve", lambda h, M=M: h.reciprocal(out=M[:, 5:6], in_=M[:, 4:5]), (kb + "den",), (kb + "g1",))
                sc.add("dve", lambda h, M=M: h.tensor_tensor(out=M[:, 6:7], in0=M[:, 3:4], in1=M[:, 5:6], op=ALU.mult),
                       (kb + "e", kb + "g1"), (kb + "g2",))
                sc.add("dve", lambda h, M=M, K1=K1: h.tensor_scalar(out=K1[:, :], in0=K1[:, :], scalar1=M[:, 5:6], scalar2=None,
                                                                    op0=ALU.mult), (kb + "k1", kb + "g1"), (kb + "k1",))
                sc.add("dve", lambda h, M=M, K1=K1, K2=K2, tt=tt: h.scalar_tensor_tensor(
                    out=comb[:, tt, :], in0=K2[:, :], scalar=M[:, 6:7], in1=K1[:, :], op0=ALU.mult, op1=ALU.add),
                    (kb + "k2", kb + "g2", kb + "k1"), (("comb", tt),))

    def ffn_phase(self, l, wsets, comb):
        sc = self.sc
        ne = len(wsets)
        hTv = self.hT_d.ap().rearrange("p (k t) -> p k t", k=KC)
        with self.scope() as es:
            gf3 = self.bc_prod(es, "gf3", l, 5, 3)
            hTt = self.sb(es, "hTt", [128, KC, 512], BF16)
            gT = self.sb(es, "gT", [128, FC, 512], BF16)
            yacc = self.sb(es, "yacc", [128, 4, D], F32)
            wgb = [self.sb(es, "wgb%d" % i, [128, KC, 256], BF16) for i in range(2)]
            wub = [self.sb(es, "wub%d" % i, [128, KC, 256], BF16) for i in range(2)]
            wdb = [self.sb(es, "wdb%d" % i, [128, 512], BF16) for i in range(4)]
            slt = [self.sb(es, "slt%d" % i, [128, 512], F32) for i in range(2)]
            xt = self.sb(es, "fxt", [128, D], F32)
            junk = self.sb(es, "fjunk", [128, D], BF16)
            st = self.sb(es, "fst", [128, 4], F32)
            nw = 0
            nwd = 0
            nsl = 0
            npb = 0
            for t4 in range(4):
                self.dma("sp", hTt[:, :, :], hTv[:, :, t4 * 512:(t4 + 1) * 512], ("hT_d",), ("hTt",))
                for e in range(ne):
                    wg, wu, wd, rk = wsets[e]
                    wgv = wg.ap().rearrange("(e k p) n -> e p k n", p=128, k=KC)[e if ne > 1 else 0]
                    wuv = wu.ap().rearrange("(e k p) n -> e p k n", p=128, k=KC)[e if ne > 1 else 0]
                    wdv = wd.ap().rearrange("(e f p) n -> e f p n", p=128, f=FC)[e if ne > 1 else 0]
                    for fg in range(FC // 2):
                        wb = nw % 2
                        nw += 1
                        self.dma("sp", wgb[wb][:, :, :], wgv[:, :, fg * 256:(fg + 1) * 256], rk, ("wgb%d" % wb,))
                        self.dma("sp", wub[wb][:, :, :], wuv[:, :, fg * 256:(fg + 1) * 256], rk, ("wub%d" % wb,))
                        for half in range(2):
                            fc = fg * 2 + half
                            ba, bu = (0, 1) if npb % 2 == 0 else (2, 3)
                            npb += 1
                            for k in range(KC):
                                sc.add("pe", lambda h, k=k, wb=wb, half=half, ba=ba: h.matmul(
                                    self.ps[ba][:, :], lhsT=wgb[wb][:, k, half * 128:(half + 1) * 128], rhs=hTt[:, k, :],
                                    start=(k == 0), stop=(k == KC - 1)), ("wgb%d" % wb, "hTt"), (("ps", ba),))
                            for k in range(KC):
                                sc.add("pe", lambda h, k=k, wb=wb, half=half, bu=bu: h.matmul(
                                    self.ps[bu][:, :], lhsT=wub[wb][:, k, half * 128:(half + 1) * 128], rhs=hTt[:, k, :],
                                    start=(k == 0), stop=(k == KC - 1)), ("wub%d" % wb, "hTt"), (("ps", bu),))
                            sb_ = nsl % 2
                            nsl += 1
                            sc.add("act", lambda h, sb_=sb_, ba=ba: h.activation(out=slt[sb_][:, :], in_=self.ps[ba][:, :],
                                                                                func=AF.Silu), (("ps", ba),), ("slt%d" % sb_,))
                            sc.add("dve", lambda h, sb_=sb_, bu=bu, fc=fc: h.tensor_tensor(
                                out=gT[:, fc, :], in0=slt[sb_][:, :], in1=self.ps[bu][:, :], op=ALU.mult),
                                ("slt%d" % sb_, ("ps", bu)), (("gT", fc),))
                    gkeys = tuple(("gT", fc) for fc in range(FC))
                    for dq in range(4):
                        for fc in range(FC):
                            db = nwd % 4
                            nwd += 1
                            self.dma("sp", wdb[db][:, :], wdv[fc][:, dq * 512:(dq + 1) * 512], rk, ("wdb%d" % db,))
                            for sub in range(4):
                                sc.add("pe", lambda h, fc=fc, sub=sub, db=db: h.matmul(
                                    self.ps[4 + sub][:, :], lhsT=gT[:, fc, sub * 128:(sub + 1) * 128], rhs=wdb[db][:, :],
                                    start=(fc == 0), stop=(fc == FC - 1)), gkeys + ("wdb%d" % db,), (("ps", 4 + sub),))
                        cs = slice(dq * 512, (dq + 1) * 512)
                        for sub in range(4):
                            tt = t4 * 4 + sub
                            if comb is None:
                                sc.add("act", lambda h, sub=sub, cs=cs: h.activation(out=yacc[:, sub, cs], in_=self.ps[4 + sub][:, :],
                                                                                    func=AF.Copy),
                                       (("ps", 4 + sub),), (("yacc", sub, dq),))
                            elif e == 0:
                                sc.add("dve", lambda h, sub=sub, cs=cs, tt=tt, e=e: h.tensor_scalar(
                                    out=yacc[:, sub, cs], in0=self.ps[4 + sub][:, :], scalar1=comb[:, tt, e:e + 1], scalar2=None,
                                    op0=ALU.mult), (("ps", 4 + sub), ("comb", tt)), (("yacc", sub, dq),))
                            else:
                                sc.add("dve", lambda h, sub=sub, cs=cs, tt=tt, e=e: h.scalar_tensor_tensor(
                                    out=yacc[:, sub, cs], in0=self.ps[4 + sub][:, :], scalar=comb[:, tt, e:e + 1],
                                    in1=yacc[:, sub, cs], op0=ALU.mult, op1=ALU.add),
                                    (("ps", 4 + sub), ("comb", tt), ("yacc", sub, dq)), (("yacc", sub, dq),))
                for sub in range(4):
                    tt = t4 * 4 + sub
                    rows = slice(tt * 128, (tt + 1) * 128)
                    yk = tuple(("yacc", sub, dq) for dq in range(4))
                    self.dma("sp", xt[:, :], self.out[rows, :], (("out", tt),), ("fxt",))
                    sc.add("act", lambda h, sub=sub: h.activation(out=junk[:, :], in_=yacc[:, sub, :], func=AF.Square,
                                                                  accum_out=st[:, 0:1]), yk, ("fst0", "fjunk"))
                    sc.add("dve", lambda h: h.tensor_scalar(out=st[:, 1:2], in0=st[:, 0:1], scalar1=1.0 / D, scalar2=EPS,
                                                             op0=ALU.mult, op1=ALU.add), ("fst0",), ("fst1",))
                    sc.add("act", lambda h: h.activation(out=st[:, 2:3], in_=st[:, 1:2], func=AF.Sqrt), ("fst1",), ("fst2",))
                    sc.add("dve", lambda h: h.reciprocal(out=st[:, 3:4], in_=st[:, 2:3]), ("fst2",), ("fst3",))
                    sc.add("dve", lambda h, sub=sub: h.scalar_tensor_tensor(
                        out=yacc[:, sub, :], in0=yacc[:, sub, :], scalar=st[:, 3:4], in1=gf3[:, :], op0=ALU.mult, op1=ALU.mult),
                        yk + ("fst3", "gf3"), yk)
                    sc.add("pool", lambda h, sub=sub: h.tensor_tensor(out=xt[:, :], in0=yacc[:, sub, :], in1=xt[:, :], op=ALU.add),
                           yk + ("fxt",), ("fxt",))
                    self.dma("sp", self.out[rows, :], xt[:, :], ("fxt",), (("out", tt),))

    def layer(self, l):
        sc = self.sc
        xsrc = self.x_in if l == 0 else self.out
        with self.scope() as es:
            hT = self.sb(es, "hT", [128, KC, TOK], BF16)
            self.norm_to_hT(l, 0, hT, lambda tt: (xsrc[tt * 128:(tt + 1) * 128, :], (("out", tt),) if l else ()))
            self.proj_phase(l, hT)
        if self.stop_after == "P%d" % l:
            self.tap("qT", self.qT, ("qT",))
            self.tap("kT", self.kT_f, ("kT_f",))
            self.tap("v", self.v_f, ("v_f",))
            self.tap("g", self.g_f, ("g_f",))
            self.done = True
            return
        self.f_phase(l)
        self.attn_phase(l)
        if self.stop_after == "A%d" % l:
            self.tap("o", self.o_d, ("o_d",))
            self.done = True
            return
        if l == 0 and self.ewg_in is not None:
            self.prep_experts()
        self.o_phase(l)
        if self.stop_after == "O%d" % l:
            self.done = True
            return
        with self.scope() as es:
            comb = None
            if l == 1:
                lg = self.sb(es, "lg", [128, 16, 8], F32)
                comb = self.sb(es, "comb", [128, 16, 8], F32)
            with self.scope() as es2:
                hT = self.sb(es2, "hT2", [128, KC, TOK], BF16)
                self.norm_to_hT(l, 1, hT, lambda tt: (self.out[tt * 128:(tt + 1) * 128, :], (("out", tt),)),
                                router=({"lg": lg} if l == 1 else None))
                self.dma("sp", self.hT_d.ap().rearrange("p (k t) -> p k t", k=KC), hT[:, :, :],
                         tuple(("hT", tt) for tt in range(16)), ("hT_d",))
            if l == 1:
                self.gating(lg, comb)
                if self.stop_after == "G1":
                    cd = self.nc.dram_tensor("tap_comb", [128, 128], F32, kind="ExternalOutput")
                    self.dma("sp", cd.ap(), comb[:, :, :].rearrange("p a b -> p (a b)"), tuple(("comb", tt) for tt in range(16)), ("out",))
                    ld = self.nc.dram_tensor("tap_lg", [128, 128], F32, kind="ExternalOutput")
                    self.dma("sp", ld.ap(), lg[:, :, :].rearrange("p a b -> p (a b)"), tuple(("lg", tt) for tt in range(16)), ("out",))
                    self.done = True
                    return
                wsets = [(self.ew_f[0], self.ew_f[1], self.ew_f[2], ("ew0", "ew1", "ew2")) for e in range(NE)]
            else:
                wsets = [(self.fw_f[0], self.fw_f[1], self.fw_f[2], ("fw0", "fw1", "fw2"))]
            self.ffn_phase(l, wsets, comb)
        if self.stop_after == "F%d" % l:
            if l == 0:
                self.tap("o", self.o_d, ("o_d",))
            self.done = True


def fm_cols():
    cols = []
    WA = 768
    qa, ka, va, fa = 0, WA, 2 * WA, 3 * WA
    qb = 3 * WA + 6
    kb, vb = qb + 768, qb + 1536
    qc = qb + 3 * 768
    kc_, vc = qc + 512, qc + 1024
    r = np.arange(128)
    perm = r.copy()
    for base in (0, 64):
        perm[base:base + 8] = np.arange(base + 8, base + 16)
        perm[base + 8:base + 16] = np.arange(base, base + 8)
    for h in range(6):
        cols += list(qa + h * 128 + r) + list(ka + h * 128 + r)
    for h in range(6):
        cols += list(qb + h * 128 + r) + list(kb + h * 128 + r)
    for h in range(4):
        cols += list(qc + h * 128 + r) + list(qc + h * 128 + perm) + list(kc_ + h * 128 + r) + list(kc_ + h * 128 + perm)
    vcols = list(va + np.arange(768)) + list(vb + np.arange(768)) + list(vc + np.arange(512))
    gcols = list(fa + np.arange(6))
    return np.array(cols), np.array(vcols), np.array(gcols)


def host_consts():
    cst = np.zeros((128, 512), np.float32)
    cst[:, 0:128] = np.eye(128)
    cst[:, 128:256] = np.eye(128)[::-1]
    p = np.arange(128)
    half = 8
    inv = (500000.0 ** (-(np.arange(half, dtype=np.float32) * 2.0 / 16))).astype(np.float32)
    invp = np.zeros(128, np.float32)
    sgn = np.zeros(128, np.float32)
    for base in (0, 64):
        invp[base:base + 8] = inv
        invp[base + 8:base + 16] = inv
        sgn[base:base + 8] = -1.0
        sgn[base + 8:base + 16] = 1.0
    cst[:, 384] = invp
    cst[:, 385] = sgn
    cst[:, 387] = 1.0
    k = np.arange(128)[:, None]
    q = np.arange(512)[None, :]
    cm = np.zeros((128, 8, 512), np.float32)
    for jj in range(4):
        cm[:, jj] = ((jj * 128 + k) <= q)
        cm[:, 4 + jj] = ((jj * 128 + k) // 64 <= q // 64)
    bm = np.zeros((128, 8, 512), np.float32)
    for jj in range(8):
        srel = 128 * jj + k - 512
        c64 = (q // 64) * 64
        bm[:, jj] = (srel >= c64 - 512) & (srel < c64 + 64)
    bm = bm[::-1].copy()
    return cst, cm.reshape(128, -1), bm.reshape(128, -1)


def core_tb(r):
    tb = np.zeros((128, 256), np.float32)
    tiles = own_tiles(r)
    BC = [list(range(0, 7)), list(range(7, 15)), list(range(15, 23)), list(range(23, 31))]
    for s in range(4):
        g = tiles[s]
        for u in range(32):
            tb[:, s * 32 + u] = 0.0 if u < g else -60000.0
        for ci, u in enumerate(BC[s]):
            tb[:, 128 + s * 8 + ci] = 0.0 if u == g - 1 else -60000.0
    return tb


def core_sel(r):
    sel = np.zeros((8, 128), np.float32)
    for s, g in enumerate(own_tiles(r)):
        sel[:, s * 32 + g] = 1.0
    return sel


def make_in_maps(inp):
    x = np.asarray(inp["x"])[0]
    pos = np.asarray(inp["positions"])[0]
    fmc, vcs, gcs = fm_cols()
    w_in = np.asarray(inp["w_in"])
    cst, cm, bm = host_consts()
    idx = np.clip(np.arange(ELEN) - 511, -REL_CLIP, REL_CLIP) + REL_CLIP
    relE = np.ascontiguousarray(np.asarray(inp["rel_bias"])[:, :, idx])
    wgate = np.zeros((2, D, 8), np.float32)
    wgate[:, :, :6] = w_in[:, :, gcs]
    bfp = np.zeros((2, 8), np.float32)
    bfp[:, :6] = np.asarray(inp["b_f"])
    mod_w = np.asarray(inp["mod_w"]).reshape(2, D, 6, 8, 256)
    mod_b = np.asarray(inp["mod_b"]).reshape(2, 6, 8, 256)
    maps = []
    for r in range(NCORES):
        tiles = own_tiles(r)
        rows = np.concatenate([np.arange(g * 512, (g + 1) * 512) for g in tiles])
        rs = slice(r * 256, (r + 1) * 256)
        m = {
            "x": np.ascontiguousarray(x[rows]),
            "pos": np.ascontiguousarray(pos[rows]).reshape(1, TOK).astype(np.int32),
            "c": np.asarray(inp["c"]).reshape(D, 1),
            "modw": np.ascontiguousarray(mod_w[:, :, :, r, :]).reshape(2, D, 1536 * 0 + 6 * 256),
            "modb": np.ascontiguousarray(mod_b[:, :, r, :]).reshape(1, 2 * 6 * 256),
            "normg": np.asarray(inp["norm_g"]).reshape(8, D),
            "wfm": np.ascontiguousarray(w_in[:, rs][:, :, fmc]),
            "wv": np.ascontiguousarray(w_in[:, rs][:, :, vcs]),
            "wgate": wgate, "bf": bfp, "relE": relE,
            "lam": np.asarray(inp["lam"]).reshape(2, 256),
            "onorm": np.asarray(inp["onorm"]),
            "wo": np.ascontiguousarray(np.asarray(inp["w_o"])[:, rs]),
            "fwg": np.ascontiguousarray(np.asarray(inp["ffn_wg"])[0, rs]),
            "fwu": np.ascontiguousarray(np.asarray(inp["ffn_wu"])[0, rs]),
            "fwd": np.ascontiguousarray(np.asarray(inp["ffn_wd"])[0, r * 896:(r + 1) * 896]),
            "rw": np.asarray(inp["router_w"])[0], "rb": np.asarray(inp["router_b"]).reshape(1, 8),
            "ewg": np.asarray(inp["exp_wg"])[0, r], "ewu": np.asarray(inp["exp_wu"])[0, r],
            "ewd": np.asarray(inp["exp_wd"])[0, r],
            "cst": cst, "cmask": cm, "bmask": bm, "tb": core_tb(r), "sel": core_sel(r),
        }
        maps.append(m)
    return maps


def kernel(**inp):
    b = Builder()
    nc = b.build()
    maps = make_in_maps(inp)
    res = run_bass_kernel_spmd(nc, maps, core_ids=list(range(NCORES)))
    out = np.zeros((1, S, D), np.float32)
    for r in range(NCORES):
        o = np.asarray(res.results[r]["out"])
        for s_, g in enumerate(own_tiles(r)):
            out[0, g * 512:(g + 1) * 512] = o[s_ * 512:(s_ + 1) * 512]
    return out
```

```python
import math
import numpy as np
import concourse.bass as bass
import concourse.mybir as mybir
from concourse.bass_utils import run_bass_kernel_spmd
from contextlib import ExitStack

F32, BF16, I32 = mybir.dt.float32, mybir.dt.bfloat16, mybir.dt.int32
AF = mybir.ActivationFunctionType
ALU = mybir.AluOpType
AX = mybir.AxisListType

import os
SKIP = set(os.environ.get('KSKIP', '').split(','))
NCORES = 8
D = 2048
S = 16384
TOK = S // NCORES
KC = D // 128
DFF = 7168
FC = DFF // 128
NE = 8
NH_A, NH_B, NH_C = 6, 6, 4
NHEAD = 16
EPS = 1e-6
REL_CLIP = 256
NFM = 16 * 256 + 4 * 256
ELEN = 1535


def own_tiles(r):
    return [r, 15 - r, 16 + r, 31 - r]


def tile_loc(g):
    if g < 8:
        return g, 0
    if g < 16:
        return 15 - g, 1
    if g < 24:
        return g - 16, 2
    return 31 - g, 3


class Op:
    __slots__ = ("eng", "fn", "deps", "is_dma", "sem", "val", "signal", "sigval", "idx", "raw")


class Sched:
    ENGS = ("pe", "act", "dve", "pool", "sp")
    NSLOT = 12

    def __init__(self, nc, es):
        self.nc = nc
        self.ops = {e: [] for e in self.ENGS}
        self.last_w = {}
        self.readers = {}
        self.esem = {e: es.enter_context(nc.semaphore("s_" + e)) for e in self.ENGS}
        self.slots = {q: [es.enter_context(nc.semaphore("d_%s%d" % (q, i))) for i in range(self.NSLOT)]
                      for q in ("sp", "pool", "act")}
        self.slot_n = {q: 0 for q in self.slots}
        self.slot_last = {q: [None] * self.NSLOT for q in self.slots}
        self.es = es
        self.ncc = 0
        self.bar_gen = 0
        self.bar_deps = []
        self.eng_gen = {e: 0 for e in self.ENGS}

    def barrier(self, full=False):
        deps = []
        for e in self.ENGS:
            if self.ops[e]:
                last = None
                for o in reversed(self.ops[e]):
                    if not o.is_dma:
                        last = o
                        break
                if last is not None:
                    deps.append(last)
        for q in self.slots:
            if q == "pool" and not full:
                continue
            for o in self.slot_last[q]:
                if o is not None:
                    deps.append(o)
        if full:
            deps += [o for o in getattr(self, "cc_ops", [])]
        self.bar_deps = deps
        self.bar_gen += 1

    def add(self, eng, fn, reads=(), writes=(), dma=False, cc=False):
        o = Op()
        o.eng, o.fn, o.is_dma, o.signal, o.sigval = eng, fn, (dma or cc), False, None
        deps = {}
        for k in reads:
            for w in self.last_w.get(k, ()):
                deps[id(w)] = (w, True)
        for k in writes:
            for w in self.last_w.get(k, ()):
                if id(w) not in deps and not (w.is_dma and (dma or cc) and not self.readers.get(k)):
                    deps[id(w)] = (w, False)
            for rd in self.readers.get(k, {}).values():
                for r_ in (rd if isinstance(rd, list) else [rd]):
                    if id(r_) not in deps:
                        deps[id(r_)] = (r_, False)
        if dma:
            q = eng
            n = self.slot_n[q]
            self.slot_n[q] = n + 1
            sl = n % self.NSLOT
            prev = self.slot_last[q][sl]
            if prev is not None:
                deps[id(prev)] = (prev, False)
            self.slot_last[q][sl] = o
            o.sem = self.slots[q][sl]
            o.val = 16 * (n // self.NSLOT + 1)
        elif cc:
            o.sem = self.es.enter_context(self.nc.semaphore("cc%d" % self.ncc))
            self.ncc += 1
            o.val = 1
        if self.eng_gen[eng] < self.bar_gen:
            self.eng_gen[eng] = self.bar_gen
            for d_ in self.bar_deps:
                if id(d_) not in deps:
                    deps[id(d_)] = (d_, True)
        if cc:
            self.cc_ops = getattr(self, "cc_ops", []) + [o]
        final = []
        for (d, raw) in deps.values():
            if d is o:
                continue
            if (not d.is_dma) and (not o.is_dma) and d.eng == eng:
                if eng == "pe" or not raw:
                    continue
            final.append(d)
            if not d.is_dma:
                d.signal = True
        o.deps = final
        for k in writes:
            prev = self.last_w.get(k, [])
            if o.is_dma and prev and prev[-1].is_dma and not self.readers.get(k):
                prev.append(o)
            else:
                self.last_w[k] = [o]
            self.readers[k] = {}
        for k in reads:
            rd = self.readers.setdefault(k, {})
            if o.is_dma:
                rd.setdefault("dma", []).append(o)
            else:
                rd[eng] = o
        o.idx = len(self.ops[eng])
        self.ops[eng].append(o)
        return o

    def emit(self, block):
        for e in self.ENGS:
            c = 0
            for o in self.ops[e]:
                if o.signal:
                    c += 1
                    o.sigval = c
        sched = self

        def run(engname, h):
            waited = {}
            for o in sched.ops[engname]:
                red = {}
                for d in o.deps:
                    kk = ("D", id(d.sem)) if d.is_dma else ("E", d.eng)
                    vv = d.val if d.is_dma else d.sigval
                    if kk not in red or vv > red[kk][0]:
                        red[kk] = (vv, d)
                for (vv, d) in red.values():
                    if d.is_dma:
                        key = ("D", id(d.sem))
                        v = d.val
                        sem = d.sem
                    else:
                        key = ("E", d.eng)
                        v = d.sigval
                        sem = sched.esem[d.eng]
                    if waited.get(key, 0) >= v:
                        continue
                    waited[key] = v
                    h.wait_ge(sem, v)
                ins = o.fn(h)
                if o.is_dma:
                    ins.then_inc(o.sem, 16 if o.val % 16 == 0 and o.val >= 16 and not _is_cc(o) else 1)
                elif o.signal:
                    ins.then_inc(sched.esem[engname], 1)

        @block.tensor
        def _(h):
            run("pe", h)

        @block.scalar
        def _(h):
            run("act", h)

        @block.vector
        def _(h):
            run("dve", h)

        @block.gpsimd
        def _(h):
            run("pool", h)

        @block.sync
        def _(h):
            run("sp", h)


def _is_cc(o):
    return o.val == 1


class Builder:
    def __init__(self, stop_after=None, taps=()):
        self.stop_after = stop_after
        self.taps = taps
        self.nc = bass.Bass("TRN2", target_bir_lowering=False)
        self.es = ExitStack()
        self.uid = 0

    def din(self, name, shape, dt=F32):
        if not hasattr(self, "in_names"):
            self.in_names = []
        if name in ("fwg", "fwu", "fwd") and self.stop_after in ("P0", "A0", "O0", "W"):
            return None
        if name in ("ewg", "ewu", "ewd") and self.stop_after is not None:
            return None
        self.in_names.append(name)
        return self.nc.dram_tensor(name, list(shape), dt, kind="ExternalInput")

    def dint(self, name, shape, dt=BF16):
        return self.nc.dram_tensor(name, list(shape), dt)

    def sb(self, es, name, shape, dt):
        esz = 2 if dt == BF16 else 4
        n = 1
        for s_ in shape[1:]:
            n *= s_
        nb = (n * esz + 63) // 64 * 64
        off = self.sb_off
        self.sb_off += nb
        self.sb_peak = max(self.sb_peak, self.sb_off)
        assert self.sb_off <= self.SB_BYTES, ("SBUF overflow", name, self.sb_off)
        v = self.big[0:shape[0], off // 2:(off + n * esz) // 2]
        if dt != BF16:
            v = v.bitcast(dt)
        if len(shape) == 3:
            v = v.rearrange("p (a b) -> p a b", a=shape[1])
        return v

    def scope(self):
        b = self

        class _S:
            def __enter__(s):
                s.mark = b.sb_off
                return s

            def __exit__(s, *a):
                b.sb_off = s.mark
                b.sc.barrier()
                return False
        return _S()

    def build(self):
        nc = self.nc
        es = self.es
        with es:
            self.sc = Sched(nc, es)
            self.SB_BYTES = 190 * 1024
            self.big = es.enter_context(nc.sbuf_tensor("big", [128, self.SB_BYTES // 2], BF16))
            self.sb_off = 0
            self.sb_peak = 0
            self.declare()
            self.ps = [es.enter_context(nc.psum_tensor("ps%d" % i, [128, 512], F32)) for i in range(8)]
            self.consts()
            self.weights_prep(0)
            self.mod_phase()
            self.weights_prep(1)
            if self.stop_after == "W":
                self.tap("mod", self.mod_f, ("mod_f",))
                self.tap("wo", self.wo_f[1], ("wo1",))
                self.done = True
            for l in range(2):
                if self.done:
                    break
                self.layer(l)
            self.finish()
            with nc.Block() as block:
                self.sc.emit(block)
        return nc

    def declare(self):
        d = self.din
        self.x_in = d("x", [TOK, D])
        self.pos_in = d("pos", [1, TOK], I32)
        self.c_in = d("c", [D, 1])
        self.modw_in = d("modw", [2, D, 1536])
        self.modb_in = d("modb", [1, 3072])
        self.normg_in = d("normg", [8, D])
        self.wfm_in = d("wfm", [2, 256, NFM])
        self.wv_in = d("wv", [2, 256, D])
        self.wgate_in = d("wgate", [2, D, 8])
        self.bf_in = d("bf", [2, 8])
        self.relE_in = d("relE", [2, 6, ELEN])
        self.lam_in = d("lam", [2, 256])
        self.onorm_in = d("onorm", [2, 3, 128])
        self.wo_in = d("wo", [2, 256, D])
        self.fwg_in = d("fwg", [256, DFF])
        self.fwu_in = d("fwu", [256, DFF])
        self.fwd_in = d("fwd", [DFF // 8, D])
        self.rw_in = d("rw", [D, 8])
        self.rb_in = d("rb", [1, 8])
        self.ewg_in = d("ewg", [D, DFF])
        self.ewu_in = d("ewu", [D, DFF])
        self.ewd_in = d("ewd", [DFF, D])
        self.cst_in = d("cst", [128, 4 * 128])
        self.cmask_in = d("cmask", [128, 8 * 512])
        self.bmask_in = d("bmask", [128, 8 * 512])
        self.tb_in = d("tb", [128, 256])
        self.sel_in = d("sel", [8, 128])
        self.out = self.nc.dram_tensor("out", [TOK, D], F32, kind="ExternalOutput")
        t = self.dint
        self.wfm_b = [t("wfm_b%d" % l, [256, NFM]) for l in range(2)]
        self.wfm_f = [t("wfm_f%d" % l, [D, NFM]) for l in range(2)]
        self.wv_b = [t("wv_b%d" % l, [256, D]) for l in range(2)]
        self.wv_f = [t("wv_f%d" % l, [D, D]) for l in range(2)]
        self.wo_b = [t("wo_b%d" % l, [256, D]) for l in range(2)]
        self.wo_f = [t("wo_f%d" % l, [D, D]) for l in range(2)]
        self.fw_b = [t("fwg_b", [256, DFF]), t("fwu_b", [256, DFF]), t("fwd_b", [DFF // 8, D])]
        self.fw_f = [t("fwg_f", [D, DFF]), t("fwu_f", [D, DFF]), t("fwd_f", [DFF, D])]
        self.ew_b = [t("ewg_b", [D, DFF]), t("ewu_b", [D, DFF]), t("ewd_b", [DFF, D])]
        self.ew_f = [t("ewg_f", [NE * D, DFF]), t("ewu_f", [NE * D, DFF]), t("ewd_f", [NE * DFF, D])]
        self.mod_b = t("mod_b", [1, 3072], F32)
        self.mod_f = t("mod_f", [8, 3072], F32)
        self.kT_b = t("kT_b", [NHEAD * 128, TOK])
        self.kT_f = t("kT_f", [NCORES * NHEAD * 128, TOK])
        self.v_b = t("v_b", [NHEAD * TOK, 128])
        self.v_f = t("v_f", [NCORES * NHEAD * TOK, 128])
        self.g_b = t("g_b", [8, TOK], F32)
        self.g_f = t("g_f", [NCORES * 8, TOK], F32)
        self.qT = t("qT", [NHEAD * 128, TOK])
        self.augK = t("augK", [6 * 8, S])
        self.augKo = t("augKo", [6 * 8, TOK])
        self.augQo = t("augQo", [6 * 8, TOK])
        self.o_d = t("o_d", [TOK, D], F32)
        self.hT_d = t("hT_d", [128, KC * TOK])
        self.done = False

    def dma(self, q, out, in_, reads=(), writes=()):
        return self.sc.add(q, lambda h, o=out, i=in_: h.dma_start(out=o, in_=i, allow_slow_non_contiguous=True), reads, writes, dma=True)

    def allgather(self, src, dst, reads, writes):
        def fn(h, s=src, d=dst):
            return h.collective_compute("AllGather", ALU.bypass, replica_groups=[list(range(NCORES))],
                                        ins=[s.ap().opt()], outs=[d.ap().opt()])
        return self.sc.add("pool", fn, reads, writes, cc=True)

    def consts(self):
        es = self.es
        self.cst = self.sb(es, "cst", [128, 512], F32)
        self.dma("sp", self.cst[:, :], self.cst_in[:, :], (), ("cst",))
        self.ident = self.cst[:, 0:128]
        self.cstb = self.sb(es, "cstb", [128, 256], BF16)
        self.sc.add("dve", lambda h: h.tensor_copy(out=self.cstb[:, :], in_=self.cst[:, 0:256]), ("cst",), ("cstb",))
        self.antiid_b = self.cstb[:, 128:256]
        self.ropec = self.cst[:, 384:512]
        self.onesb = self.sb(es, "onesb", [8, 2048], BF16)
        self.sc.add("pool", lambda h: h.memset(self.onesb[:, :], 1.0), (), ("onesb",))
        self.onesf = self.sb(es, "onesf", [8, 2048], F32)
        self.sc.add("pool", lambda h: h.memset(self.onesf[:, :], 1.0), (), ("onesf",))
        self.epsc = self.sb(es, "epsc", [128, 2], F32)
        self.sc.add("pool", lambda h: h.memset(self.epsc[:, 0:1], EPS), (), ("epsc",))
        self.sc.add("pool", lambda h: h.memset(self.epsc[:, 1:2], 1.0), (), ("epsc1",))

    def cast_to(self, src, dst, rows, key, piece=256):
        for r0 in range(0, rows, piece):
            r1 = min(rows, r0 + piece)
            self.dma("pool", dst[r0:r1, :], src[r0:r1, :], (), (key + str(r0),))
        return [key + str(r0) for r0 in range(0, rows, piece)]

    def weights_prep(self, stage):
        def prep(src, bounce, full, rows, key, piece=256):
            ks = self.cast_to(src, bounce, rows, key + "_b", piece)
            self.allgather(bounce, full, ks, (key,))
        if stage == 0:
            prep(self.wfm_in[0], self.wfm_b[0], self.wfm_f[0], 256, "wfm0")
            return
        for l in range(2):
            if l == 1:
                prep(self.wfm_in[l], self.wfm_b[l], self.wfm_f[l], 256, "wfm%d" % l)
            prep(self.wv_in[l], self.wv_b[l], self.wv_f[l], 256, "wv%d" % l)
            prep(self.wo_in[l], self.wo_b[l], self.wo_f[l], 256, "wo%d" % l)
            if l == 0 and self.fwg_in is not None:
                prep(self.fwg_in, self.fw_b[0], self.fw_f[0], 256, "fw0")
                prep(self.fwu_in, self.fw_b[1], self.fw_f[1], 256, "fw1")
                prep(self.fwd_in, self.fw_b[2], self.fw_f[2], DFF // 8, "fw2", 448)
        self.prep_experts = lambda: [
            prep(self.ewg_in, self.ew_b[0], self.ew_f[0], D, "ew0", 128),
            prep(self.ewu_in, self.ew_b[1], self.ew_f[1], D, "ew1", 128),
            prep(self.ewd_in, self.ew_b[2], self.ew_f[2], DFF, "ew2", 512)]

    def mod_phase(self):
        nc, sc = self.nc, self.sc
        with self.scope() as es:
            cT = self.sb(es, "cT", [128, KC], F32)
            self.dma("sp", cT[:, :], self.c_in.ap().rearrange("(k p) o -> p (k o)", p=128), (), ("cT",))
            cond = self.sb(es, "cond", [128, KC], F32)
            sc.add("act", lambda h: h.activation(out=cond[:, :], in_=cT[:, :], func=AF.Silu), ("cT",), ("cond",))
            mrow = self.sb(es, "mrow", [1, 3072], F32)
            mbias = self.sb(es, "mbias", [1, 3072], F32)
            self.dma("sp", mbias[:, :], self.modb_in[:, :], (), ("mbias",))
            wb = [self.sb(es, "mw%d" % i, [128, KC, 512], F32) for i in range(2)]
            n = 0
            for l in range(2):
                for cc in range(3):
                    b = n % 2
                    self.dma("sp", wb[b][:, :, :],
                             self.modw_in[l].rearrange("(k p) n -> p k n", p=128)[:, :, cc * 512:(cc + 1) * 512],
                             (), ("mw%d" % b,))
                    pk = ("ps", n % 2)
                    for k in range(KC):
                        sc.add("pe", lambda h, k=k, b=b, n=n: h.matmul(
                            self.ps[n % 2][0:1, :], lhsT=cond[:, k:k + 1], rhs=wb[b][:, k, :],
                            start=(k == 0), stop=(k == KC - 1)), ("cond", "mw%d" % b), (pk,))
                    col = l * 1536 + cc * 512
                    sc.add("dve", lambda h, n=n, col=col: h.tensor_tensor(
                        out=mrow[:, col:col + 512], in0=self.ps[n % 2][0:1, :], in1=mbias[:, col:col + 512],
                        op=ALU.add), (pk, "mbias"), ("mrow%d" % n,))
                    n += 1
            self.dma("sp", self.mod_b[:, :], mrow[:, :], tuple("mrow%d" % i for i in range(6)), ("mod_b",))
            self.allgather(self.mod_b, self.mod_f, ("mod_b",), ("mod_f",))
        es = self.es
        self.modv = {}

    def mod_vec_src(self, l, c):
        return self.mod_f.ap().rearrange("r (l c i) -> l c r i", l=2, c=6)[l, c]

    def load_fm_vec(self, es, name, src_r_i, key_r, key_w):
        t = self.sb(es, name, [128, KC], F32)
        for r in range(8):
            self.dma("sp", t[:, 2 * r:2 * r + 2], src_r_i[r].rearrange("(j p) -> p j", p=128), key_r, (key_w,))
        return t

    def load_bc_vec(self, es, name, src_r_i, key_r, key_w):
        t = self.sb(es, name, [128, D], F32)
        self.dma("sp", t[:, :].rearrange("p (r i) -> p r i", r=8),
                 src_r_i.partition_broadcast(128), key_r, (key_w,))
        return t

    def normg_src(self, l, j):
        return self.normg_in[l * 4 + j:l * 4 + j + 1, :].rearrange("o (r i) -> (o r) i", r=8)

    def finish(self):
        sc = self.sc
        sc.barrier(full=True)
        sc.add("sp", lambda h: h.nop(), ("out",), ())

    def tap(self, name, src, key):
        shp = list(src.shape)
        rows = min(shp[0], max(128, (2 << 20) // (shp[1] * 4)))
        shp[0] = min(shp[0], 8 * rows)
        o = self.nc.dram_tensor("tap_" + name, shp, src.dtype, kind="ExternalOutput")
        for r0 in range(0, shp[0], rows):
            self.dma("sp", o[r0:r0 + rows, :], src[r0:r0 + rows, :], key, ("out",))

    def rms_rstd(self, es_bufs, src_ap, n, kr, tag, width):
        sc = self.sc
        junk, ss, sd, rstd = es_bufs
        sc.add("act", lambda h: h.activation(out=junk[:, 0:width], in_=src_ap, func=AF.Square, accum_out=ss[:, 0:1]),
               kr, (tag + "ss", tag + "junk"))
        sc.add("dve", lambda h: h.tensor_scalar(out=sd[:, 0:1], in0=ss[:, 0:1], scalar1=1.0 / n, scalar2=EPS,
                                                 op0=ALU.mult, op1=ALU.add), (tag + "ss",), (tag + "sd",))
        sc.add("act", lambda h: h.activation(out=sd[:, 1:2], in_=sd[:, 0:1], func=AF.Sqrt), (tag + "sd",), (tag + "sd2",))
        sc.add("dve", lambda h: h.reciprocal(out=rstd[:, 0:1], in_=sd[:, 1:2]), (tag + "sd2",), (tag + "rstd",))
        return tag + "rstd"

    def norm_to_hT(self, l, which, hT, xsrc_fn, router=None):
        sc = self.sc
        with self.scope() as es:
            j0 = 0 if which == 0 else 2
            c0 = 0 if which == 0 else 3
            gv = self.load_fm_vec(es, "gv", self.normg_src(l, j0), (), "gv")
            scv = self.load_fm_vec(es, "scv", self.mod_vec_src(l, c0 + 1), ("mod_f",), "scv")
            shv = self.load_fm_vec(es, "shv", self.mod_vec_src(l, c0), ("mod_f",), "shv")
            av = self.sb(es, "av", [128, KC], F32)
            sc.add("dve", lambda h: h.scalar_tensor_tensor(out=av[:, :], in0=scv[:, :], scalar=1.0, in1=gv[:, :],
                                                            op0=ALU.add, op1=ALU.mult), ("gv", "scv"), ("av",))
            xt = [self.sb(es, "xt%d" % i, [128, D], F32) for i in range(2)]
            xn = [self.sb(es, "xn%d" % i, [128, D], F32) for i in range(2)]
            junk = self.sb(es, "junk", [128, D], BF16)
            st = [[self.sb(es, "st%d_%d" % (i, j), [128, 2], F32) for j in range(3)] for i in range(2)]
            if router is not None:
                hf = [self.sb(es, "hf%d" % i, [128, 128], F32) for i in range(4)]
                hhi = [self.sb(es, "hhi%d" % i, [128, 128], BF16) for i in range(4)]
                hlo = [self.sb(es, "hlo%d" % i, [128, 128], BF16) for i in range(4)]
                rw = self.sb(es, "rw", [128, KC, 8], F32)
                self.dma("sp", rw[:, :, :], self.rw_in.ap().rearrange("(k p) e -> p k e", p=128), (), ("rw",))
                rhi = self.sb(es, "rhi", [128, KC, 128], BF16)
                rlo = self.sb(es, "rlo", [128, KC, 128], BF16)
                sc.add("pool", lambda h: h.memset(rhi[:, :, :], 0.0), (), ("rhi",))
                sc.add("pool", lambda h: h.memset(rlo[:, :, :], 0.0), (), ("rlo",))
                sc.add("dve", lambda h: h.tensor_copy(out=rhi[:, :, 0:8], in_=rw[:, :, :]), ("rw", "rhi"), ("rhi",))
                sc.add("dve", lambda h: h.tensor_tensor(out=rw[:, :, :], in0=rw[:, :, :], in1=rhi[:, :, 0:8], op=ALU.subtract),
                       ("rw", "rhi"), ("rw",))
                sc.add("dve", lambda h: h.tensor_copy(out=rlo[:, :, 0:8], in_=rw[:, :, :]), ("rw", "rlo"), ("rlo",))
                rbb = self.sb(es, "rbb", [128, 8], F32)
                self.dma("sp", rbb[:, :], self.rb_in[0:1, :].partition_broadcast(128), (), ("rbb",))
            nb = 0
            for tt in range(16):
                b = tt % 2
                src, kr = xsrc_fn(tt)
                self.dma("sp", xt[b][:, :], src, kr, ("xt%d" % b,))
                kk = self.rms_rstd((junk, st[b][0], st[b][1], st[b][2]), xt[b][:, :], D, ("xt%d" % b,), "n%d" % b, D)
                sc.add("dve", lambda h, b=b: h.tensor_scalar(out=xn[b][:, :], in0=xt[b][:, :], scalar1=st[b][2][:, 0:1],
                                                             scalar2=None, op0=ALU.mult), ("xt%d" % b, kk), ("xn%d" % b,))
                for q4 in range(4):
                    bank = 4 + (nb % 4) if router is not None else (nb % 8)
                    nb += 1
                    for j in range(4):
                        kc = q4 * 4 + j
                        sc.add("pe", lambda h, b=b, kc=kc, j=j, bank=bank: h.transpose(
                            out=self.ps[bank][:, j * 128:(j + 1) * 128], in_=xn[b][:, kc * 128:(kc + 1) * 128],
                            identity=self.ident), ("xn%d" % b, "cst"), (("ps", bank),))
                    for j in range(4):
                        kc = q4 * 4 + j
                        sc.add("act", lambda h, kc=kc, j=j, bank=bank, tt=tt: h.activation(
                            out=hT[:, kc, tt * 128:(tt + 1) * 128], in_=self.ps[bank][:, j * 128:(j + 1) * 128],
                            func=AF.Identity, scale=av[:, kc:kc + 1], bias=shv[:, kc:kc + 1]),
                            (("ps", bank), "av", "shv"), (("hT", tt),))
                    if router is not None and "rt" not in SKIP:
                        for j in range(4):
                            kc = q4 * 4 + j
                            hb = kc % 4
                            sc.add("act", lambda h, kc=kc, j=j, bank=bank, hb=hb: h.activation(
                                out=hf[hb][:, :], in_=self.ps[bank][:, j * 128:(j + 1) * 128], func=AF.Identity,
                                scale=av[:, kc:kc + 1], bias=shv[:, kc:kc + 1]),
                                (("ps", bank), "av", "shv"), ("hf%d" % hb,))
                            sc.add("dve", lambda h, hb=hb: h.tensor_copy(out=hhi[hb][:, :], in_=hf[hb][:, :]),
                                   ("hf%d" % hb,), ("hhi%d" % hb,))
                            sc.add("dve", lambda h, hb=hb: h.tensor_tensor(out=hf[hb][:, :], in0=hf[hb][:, :], in1=hhi[hb][:, :],
                                                                           op=ALU.subtract),
                                   ("hf%d" % hb, "hhi%d" % hb), ("hf%d" % hb,))
                            sc.add("dve", lambda h, hb=hb: h.tensor_copy(out=hlo[hb][:, :], in_=hf[hb][:, :]),
                                   ("hf%d" % hb,), ("hlo%d" % hb,))
                            sc.add("pe", lambda h, kc=kc, hb=hb, tt=tt: h.matmul(
                                self.ps[tt % 2][:, 0:128], lhsT=hhi[hb][:, :], rhs=rhi[:, kc, :],
                                start=(kc == 0), stop=False), ("hhi%d" % hb, "rhi"), (("ps", tt % 2),))
                            sc.add("pe", lambda h, kc=kc, hb=hb, tt=tt: h.matmul(
                                self.ps[tt % 2][:, 0:128], lhsT=hhi[hb][:, :], rhs=rlo[:, kc, :],
                                start=False, stop=False), ("hhi%d" % hb, "rlo"), (("ps", tt % 2),))
                            sc.add("pe", lambda h, kc=kc, hb=hb, tt=tt: h.matmul(
                                self.ps[tt % 2][:, 0:128], lhsT=hlo[hb][:, :], rhs=rhi[:, kc, :],
                                start=False, stop=(kc == KC - 1)), ("hlo%d" % hb, "rhi"), (("ps", tt % 2),))
                if router is not None and "lg" not in SKIP:
                    lg = router["lg"]
                    sc.add("dve", lambda h, tt=tt: h.tensor_tensor(out=lg[:, tt, :], in0=self.ps[tt % 2][:, 0:8],
                                                                   in1=rbb[:, :], op=ALU.add),
                           (("ps", tt % 2), "rbb"), (("lg", tt),))

    def rope_tables(self, es):
        sc = self.sc
        posi = self.sb(es, "posi", [128, TOK], I32)
        self.dma("sp", posi[:, :], self.pos_in[0:1, :].partition_broadcast(128), (), ("posi",))
        ang = self.sb(es, "ang", [128, TOK], F32)
        nr = self.sb(es, "nr", [128, TOK], F32)
        cosT = self.sb(es, "cosT", [128, TOK], F32)
        sinT = self.sb(es, "sinT", [128, TOK], F32)
        MAGIC = 12582912.0
        PI = math.pi
        LO = 2 * math.pi - 6.28125
        sc.add("dve", lambda h: h.tensor_copy(out=nr[:, :], in_=posi[:, :]), ("posi",), ("nr",))
        sc.add("dve", lambda h: h.tensor_scalar(out=ang[:, :], in0=nr[:, :], scalar1=self.ropec[:, 0:1], scalar2=None,
                                                 op0=ALU.mult), ("nr", "cst"), ("ang",))
        sc.add("dve", lambda h: h.tensor_scalar(out=nr[:, :], in0=ang[:, :], scalar1=1.0 / (2 * PI), scalar2=MAGIC,
                                                 op0=ALU.mult, op1=ALU.add), ("ang",), ("nr",))
        sc.add("dve", lambda h: h.tensor_scalar(out=nr[:, :], in0=nr[:, :], scalar1=MAGIC, scalar2=None,
                                                 op0=ALU.subtract), ("nr",), ("nr",))
        sc.add("dve", lambda h: h.scalar_tensor_tensor(out=ang[:, :], in0=nr[:, :], scalar=-6.28125, in1=ang[:, :],
                                                        op0=ALU.mult, op1=ALU.add), ("nr", "ang"), ("ang",))
        sc.add("dve", lambda h: h.scalar_tensor_tensor(out=ang[:, :], in0=nr[:, :], scalar=-LO, in1=ang[:, :],
                                                        op0=ALU.mult, op1=ALU.add), ("nr", "ang"), ("ang",))
        sc.add("dve", lambda h: h.tensor_scalar(out=ang[:, :], in0=ang[:, :], scalar1=PI, scalar2=-PI,
                                                 op0=ALU.min, op1=ALU.max), ("ang",), ("ang",))
        sc.add("act", lambda h: h.activation(out=sinT[:, :], in_=ang[:, :], func=AF.Sin), ("ang",), ("sinT",))
        sc.add("dve", lambda h: h.tensor_scalar(out=sinT[:, :], in0=sinT[:, :], scalar1=self.ropec[:, 1:2], scalar2=None,
                                                 op0=ALU.mult), ("sinT", "cst"), ("sinT",))
        sc.add("dve", lambda h: h.scalar_tensor_tensor(out=nr[:, :], in0=ang[:, :], scalar=-1.0, in1=ang[:, :],
                                                        op0=ALU.mult, op1=ALU.max), ("ang",), ("nr",))
        sc.add("dve", lambda h: h.tensor_scalar(out=nr[:, :], in0=nr[:, :], scalar1=-1.0, scalar2=PI / 2,
                                                 op0=ALU.mult, op1=ALU.add), ("nr",), ("nr",))
        sc.add("act", lambda h: h.activation(out=cosT[:, :], in_=nr[:, :], func=AF.Sin), ("nr",), ("cosT",))
        return cosT, sinT

    def proj_phase(self, l, hT):
        sc = self.sc
        hkeys = tuple(("hT", tt) for tt in range(16))
        with self.scope() as es:
            cosT, sinT = self.rope_tables(es)
            wt = [self.sb(es, "wt%d" % i, [128, KC, 512], BF16) for i in range(2)]
            stg = [self.sb(es, "stg%d" % i, [128, 512], BF16) for i in range(4)]
            rt = [self.sb(es, "rt%d" % i, [128, 512], F32) for i in range(2)]
            nstg = 0
            nbank = 0
            wsrc = self.wfm_f[l].ap().rearrange("(k p) n -> p k n", p=128)
            wgf = self.sb(es, "wgf", [128, KC, 8], F32)
            self.dma("sp", wgf[:, :, :], self.wgate_in[l].rearrange("(k p) e -> p k e", p=128), (), ("wgf",))
            wgb = self.sb(es, "wgb", [128, KC, 8], BF16)
            sc.add("dve", lambda h: h.tensor_copy(out=wgb[:, :, :], in_=wgf[:, :, :]), ("wgf",), ("wgb",))
            nb_f = self.sb(es, "nbf", [8, 1], F32)
            self.dma("sp", nb_f[:, :], self.bf_in[l:l + 1, :].rearrange("o e -> e o"), (), ("nbf",))
            sc.add("dve", lambda h: h.tensor_scalar(out=nb_f[:, :], in0=nb_f[:, :], scalar1=-1.0, scalar2=None,
                                                     op0=ALU.mult), ("nbf",), ("nbf",))
            gt = self.sb(es, "gt", [8, TOK], F32)
            for ts in range(4):
                bank = nbank % 8
                nbank += 1
                for k in range(KC):
                    sc.add("pe", lambda h, k=k, ts=ts, bank=bank: h.matmul(
                        self.ps[bank][0:8, :], lhsT=wgb[:, k, :], rhs=hT[:, k, ts * 512:(ts + 1) * 512],
                        start=(k == 0), stop=(k == KC - 1)), hkeys + ("wgb",), (("ps", bank),))
                sc.add("act", lambda h, ts=ts, bank=bank: h.activation(
                    out=gt[:, ts * 512:(ts + 1) * 512], in_=self.ps[bank][0:8, :], func=AF.Exp, scale=-1.0,
                    bias=nb_f[:, 0:1]), (("ps", bank), "nbf"), (("gt", ts),))
            sc.add("act", lambda h: h.activation(out=gt[:, :], in_=gt[:, :], func=AF.Ln, bias=self.epsc[0:8, 1:2]),
                   tuple(("gt", ts) for ts in range(4)) + ("epsc1",), ("gtl",))
            self.dma("sp", self.g_b[:, :], gt[:, :], ("gtl",), ("g_b",))
            self.allgather(self.g_b, self.g_f, ("g_b",), ("g_f",))
            for gi in range(10):
                wb = gi % 2
                self.dma("sp", wt[wb][:, :, :], wsrc[:, :, gi * 512:(gi + 1) * 512], ("wfm%d" % l,), ("wt%d" % wb,))
                for ts in range(4):
                    banks = []
                    for jj in range(4):
                        bank = nbank % 8
                        nbank += 1
                        banks.append(bank)
                        for k in range(KC):
                            sc.add("pe", lambda h, k=k, ts=ts, bank=bank, jj=jj, wb=wb: h.matmul(
                                self.ps[bank][:, :], lhsT=wt[wb][:, k, jj * 128:(jj + 1) * 128],
                                rhs=hT[:, k, ts * 512:(ts + 1) * 512], start=(k == 0), stop=(k == KC - 1)),
                                hkeys + ("wt%d" % wb,), (("ps", bank),))
                    tsl = slice(ts * 512, (ts + 1) * 512)
                    if gi < 6:
                        for jj in range(4):
                            hh = gi * 2 + jj // 2
                            dst = (self.qT if jj % 2 == 0 else self.kT_b)
                            s_ = nstg % 4
                            nstg += 1
                            eng = "act" if jj % 2 == 0 else "dve"
                            if eng == "act":
                                sc.add("act", lambda h, s_=s_, bank=banks[jj]: h.activation(
                                    out=stg[s_][:, :], in_=self.ps[bank][:, :], func=AF.Copy),
                                    (("ps", banks[jj]),), ("stg%d" % s_,))
                            else:
                                sc.add("dve", lambda h, s_=s_, bank=banks[jj]: h.tensor_copy(
                                    out=stg[s_][:, :], in_=self.ps[bank][:, :]), (("ps", banks[jj]),), ("stg%d" % s_,))
                            self.dma("sp", dst[hh * 128:(hh + 1) * 128, tsl], stg[s_][:, :], ("stg%d" % s_,),
                                     ("qT" if jj % 2 == 0 else "kT_b",))
                    else:
                        hh = 12 + (gi - 6)
                        for pr in range(2):
                            b0, b1 = banks[2 * pr], banks[2 * pr + 1]
                            s_ = nstg % 4
                            nstg += 1
                            sc.add("dve", lambda h, b0=b0, ts=ts: h.tensor_tensor(
                                out=rt[0][:, :], in0=self.ps[b0][:, :], in1=cosT[:, ts * 512:(ts + 1) * 512], op=ALU.mult),
                                (("ps", b0), "cosT"), ("rt0",))
                            sc.add("dve", lambda h, b1=b1, ts=ts: h.tensor_tensor(
                                out=rt[1][:, :], in0=self.ps[b1][:, :], in1=sinT[:, ts * 512:(ts + 1) * 512], op=ALU.mult),
                                (("ps", b1), "sinT"), ("rt1",))
                            sc.add("pool", lambda h, s_=s_: h.tensor_tensor(
                                out=stg[s_][:, :], in0=rt[0][:, :], in1=rt[1][:, :], op=ALU.add),
                                ("rt0", "rt1"), ("stg%d" % s_,))
                            dst = self.qT if pr == 0 else self.kT_b
                            self.dma("sp", dst[hh * 128:(hh + 1) * 128, tsl], stg[s_][:, :], ("stg%d" % s_,),
                                     ("qT" if pr == 0 else "kT_b",))
            self.allgather(self.kT_b, self.kT_f, ("kT_b",), ("kT_f",))
            vsrc = self.wv_f[l].ap().rearrange("(k p) n -> p k n", p=128)
            vdst = self.v_b.ap().rearrange("(h t) d -> t h d", h=NHEAD)
            for vc in range(4):
                wb = vc % 2
                self.dma("sp", wt[wb][:, :, :], vsrc[:, :, vc * 512:(vc + 1) * 512], ("wv%d" % l,), ("wt%d" % wb,))
                for tt in range(16):
                    bank = nbank % 8
                    nbank += 1
                    for k in range(KC):
                        sc.add("pe", lambda h, k=k, tt=tt, bank=bank, wb=wb: h.matmul(
                            self.ps[bank][:, :], lhsT=hT[:, k, tt * 128:(tt + 1) * 128], rhs=wt[wb][:, k, :],
                            start=(k == 0), stop=(k == KC - 1)), hkeys + ("wt%d" % wb,), (("ps", bank),))
                    s_ = nstg % 4
                    nstg += 1
                    if tt % 2 == 0:
                        sc.add("act", lambda h, s_=s_, bank=bank: h.activation(
                            out=stg[s_][:, :], in_=self.ps[bank][:, :], func=AF.Copy), (("ps", bank),), ("stg%d" % s_,))
                    else:
                        sc.add("dve", lambda h, s_=s_, bank=bank: h.tensor_copy(
                            out=stg[s_][:, :], in_=self.ps[bank][:, :]), (("ps", bank),), ("stg%d" % s_,))
                    self.dma("sp", vdst[tt * 128:(tt + 1) * 128, vc * 4:(vc + 1) * 4, :],
                             stg[s_][:, :].rearrange("p (h d) -> p h d", h=4), ("stg%d" % s_,), ("v_b",))
            self.allgather(self.v_b, self.v_f, ("v_b",), ("v_f",))

    def f_phase(self, l):
        sc = self.sc
        SQ = math.sqrt(128.0)
        with self.scope() as es:
            Gall = self.sb(es, "Gall", [8, 8, 2048], F32)
            self.dma("sp", Gall[:, :, :], self.g_f.ap().rearrange("(r h) t -> h r t", r=8), ("g_f",), ("Gall",))
            Gown = self.sb(es, "Gown", [8, 2048], F32)
            self.dma("sp", Gown[:, :], self.g_b[:, :], ("g_b",), ("Gown",))
            cum = self.sb(es, "cum", [8, 8, 2048], F32)
            cown = self.sb(es, "cown", [8, 2048], F32)
            cex = self.sb(es, "cex", [8, 32], F32)
            selb = self.sb(es, "selb", [8, 128], F32)
            self.dma("sp", selb[:, :], self.sel_in[:, :], (), ("selb",))
            pown = self.sb(es, "pown", [8, 4], F32)
            tmp = self.sb(es, "ftmp", [8, 32], F32)
            sc.add("pool", lambda h: h.memset(cex[:, :], 0.0), (), ("cex",))
            for u in range(32):
                rk, sl = tile_loc(u)
                init = 0.0 if u == 0 else cex[:, u:u + 1]
                sc.add("dve", lambda h, rk=rk, sl=sl, init=init: h.tensor_tensor_scan(
                    out=cum[:, rk, sl * 512:(sl + 1) * 512], data0=self.onesf[:, 0:512],
                    data1=Gall[:, rk, sl * 512:(sl + 1) * 512], initial=init, op0=ALU.mult, op1=ALU.add),
                    ("Gall", "onesf", "cex"), (("cum", rk),))
                if u < 31:
                    sc.add("dve", lambda h, rk=rk, sl=sl, u=u: h.tensor_copy(
                        out=cex[:, u + 1:u + 2], in_=cum[:, rk, sl * 512 + 511:sl * 512 + 512]),
                        (("cum", rk),), ("cex",))
            for s in range(4):
                sc.add("dve", lambda h, s=s: h.tensor_tensor(out=tmp[:, :], in0=cex[:, :], in1=selb[:, s * 32:(s + 1) * 32],
                                                             op=ALU.mult), ("cex", "selb"), ("ftmp",))
                sc.add("dve", lambda h, s=s: h.reduce_sum(out=pown[:, s:s + 1], in_=tmp[:, :], axis=AX.X),
                       ("ftmp",), ("pown",))
                sc.add("dve", lambda h, s=s: h.tensor_tensor_scan(
                    out=cown[:, s * 512:(s + 1) * 512], data0=self.onesf[:, 0:512], data1=Gown[:, s * 512:(s + 1) * 512],
                    initial=pown[:, s:s + 1], op0=ALU.mult, op1=ALU.add), ("Gown", "onesf", "pown"), ("cown",))
            hb = [self.sb(es, "hb%d" % i, [8, 2048], BF16) for i in range(4)]
            nhb = [0]
            aK = self.augK.ap().rearrange("(h r) (k t) -> h r k t", r=6, k=8)
            aKo = self.augKo.ap().rearrange("(h r) t -> h r t", r=6)
            aQo = self.augQo.ap().rearrange("(h r) t -> h r t", r=6)

            def split_store(src, kr, kname, dstK, dstQ):
                sc.add("dve", lambda h: h.tensor_scalar(out=src, in0=src, scalar1=SQ, scalar2=None, op0=ALU.mult),
                       (kr,), (kr,))
                for i in range(3):
                    b = nhb[0] % 4
                    nhb[0] += 1
                    sc.add("dve", lambda h, b=b: h.tensor_copy(out=hb[b][:, :], in_=src), (kr,), ("hb%d" % b,))
                    self.dma("sp", dstK(3 + i), hb[b][:, :], ("hb%d" % b,), (kname,))
                    if i < 2:
                        sc.add("dve", lambda h, b=b: h.tensor_tensor(out=src, in0=src, in1=hb[b][:, :], op=ALU.subtract),
                               (kr, "hb%d" % b), (kr,))
                    if dstQ is not None:
                        b2 = nhb[0] % 4
                        nhb[0] += 1
                        sc.add("dve", lambda h, b=b, b2=b2: h.tensor_scalar(out=hb[b2][:, :], in0=hb[b][:, :], scalar1=-1.0,
                                                                            scalar2=None, op0=ALU.mult),
                               ("hb%d" % b,), ("hb%d" % b2,))
                        self.dma("sp", dstQ(i), hb[b2][:, :], ("hb%d" % b2,), ("augQo",))
            for rk in range(8):
                split_store(cum[:, rk, :], ("cum", rk), "augK", lambda row, rk=rk: aK[:, row, rk, :], None)
            split_store(cown[:, :], "cown", "augKo", lambda row: aKo[:, row, :], lambda row: aQo[:, row, :])
            if l == 0:
                for row in range(3):
                    for rk in range(8):
                        self.dma("sp", aK[:, row, rk, :], self.onesb[:, :], ("onesb",), ("augK",))
                    self.dma("sp", aKo[:, row, :], self.onesb[:, :], ("onesb",), ("augKo",))
                    self.dma("sp", aQo[:, 3 + row, :], self.onesb[:, :], ("onesb",), ("augQo",))

    def attn_phase(self, l):
        sc = self.sc
        SQ = math.sqrt(128.0)
        NEG = 3.0e5
        lam_init = 0.8 - 0.6 * math.exp(-0.3 * l)
        with self.scope() as es:
            tb = self.sb(es, "tb", [128, 256], F32)
            self.dma("sp", tb[:, :], self.tb_in[:, :], (), ("tb",))
            negC = self.sb(es, "negC", [128, 8, 512], BF16)
            negB = self.sb(es, "negB", [128, 8, 512], BF16)
            _ms = self.scope()
            _ms.__enter__()
            stage = self.sb(es, "mstage", [128, 8, 512], F32)
            self.dma("sp", stage[:, :, :], self.cmask_in.ap().rearrange("p (j q) -> p j q", j=8), (), ("mstage",))
            sc.add("dve", lambda h: h.tensor_scalar(out=negC[:, :, :], in0=stage[:, :, :], scalar1=-1.0, scalar2=NEG,
                                                     op0=ALU.add, op1=ALU.mult), ("mstage",), ("negC",))
            self.dma("sp", stage[:, :, :], self.bmask_in.ap().rearrange("p (j q) -> p j q", j=8), (), ("mstage",))
            sc.add("dve", lambda h: h.tensor_scalar(out=negB[:, :, :], in0=stage[:, :, :], scalar1=-1.0, scalar2=NEG,
                                                     op0=ALU.add, op1=ALU.mult), ("mstage",), ("negB",))
            _ms.__exit__()
            identb = self.cstb[:, 0:128]
            onw = self.sb(es, "onw", [128, 3, 128], F32)
            self.dma("sp", onw[:, :, :], self.onorm_in[l].partition_broadcast(128), (), ("onw",))
            sc.add("dve", lambda h: h.tensor_scalar(out=onw[:, 2, :], in0=onw[:, 2, :], scalar1=1.0 - lam_init,
                                                     scalar2=None, op0=ALU.mult), ("onw",), ("onw",))
            lamb = self.sb(es, "lamb", [128, 256], F32)
            self.dma("sp", lamb[:, :], self.lam_in[l:l + 1, :].partition_broadcast(128), (), ("lamb",))
            lt = self.sb(es, "lt", [128, 128], F32)
            ls = self.sb(es, "ls", [128, 4], F32)
            for i in range(2):
                sc.add("dve", lambda h, i=i: h.tensor_tensor(out=lt[:, i * 64:(i + 1) * 64], in0=lamb[:, i * 128:i * 128 + 64],
                                                             in1=lamb[:, i * 128 + 64:i * 128 + 128], op=ALU.mult),
                       ("lamb",), ("lt",))
                sc.add("dve", lambda h, i=i: h.reduce_sum(out=ls[:, i:i + 1], in_=lt[:, i * 64:(i + 1) * 64], axis=AX.X),
                       ("lt",), ("ls",))
            sc.add("act", lambda h: h.activation(out=ls[:, 2:4], in_=ls[:, 0:2], func=AF.Exp), ("ls",), ("ls2",))
            nlam = self.sb(es, "nlam", [128, 1], F32)
            sc.add("dve", lambda h: h.scalar_tensor_tensor(out=nlam[:, 0:1], in0=ls[:, 3:4], scalar=-lam_init, in1=ls[:, 2:3],
                                                            op0=ALU.add, op1=ALU.subtract), ("ls2",), ("nlam",))
            kt = self.sb(es, "kt", [128, 8, 2048], BF16)
            kto = self.sb(es, "kto", [128, 2048], BF16)
            vt = self.sb(es, "vt", [128, 128, 129], BF16)
            vto = self.sb(es, "vto", [128, 16, 129], BF16)
            sc.add("pool", lambda h: h.memset(vt[:, :, 128:129], 1.0), (), ("vt1",))
            sc.add("pool", lambda h: h.memset(vto[:, :, 128:129], 1.0), (), ("vto1",))
            qt = self.sb(es, "qt", [128, 2048], BF16)
            pT = [self.sb(es, "pT%d" % i, [128, 512], BF16) for i in range(6)]
            fin = [self.sb(es, "fin%d" % i, [128, 8], F32) for i in range(2)]
            onb = [self.sb(es, "onb%d" % i, [128, 128], F32) for i in range(2)]
            ofb = [self.sb(es, "ofb%d" % i, [128, 128], F32) for i in range(2)]
            junk = self.sb(es, "ajunk", [128, 128], BF16)
            kTf = self.kT_f.ap().rearrange("(r h d) t -> d r h t", r=8, h=NHEAD)
            vf = self.v_f.ap().rearrange("(r h k p) d -> p r h k d", r=8, h=NHEAD, k=16)
            vb_ = self.v_b.ap().rearrange("(h k p) d -> p h k d", h=NHEAD, k=16)
            aK = self.augK.ap().rearrange("(h r) (k t) -> h r k t", r=6, k=8)
            aKo = self.augKo.ap().rearrange("(h r) t -> h r t", r=6)
            aQo = self.augQo.ap().rearrange("(h r) t -> h r t", r=6)
            npt = 0
            nsb = 0
            nfin = 0
            MAXU = [7, 15, 23, 31]
            BC = [list(range(0, 7)), list(range(7, 15)), list(range(15, 23)), list(range(23, 31))]
            gsc = None
            for hh in range(NHEAD):
                typ = "A" if hh < 6 else ("B" if hh < 12 else "C")
                scale = 64.0 ** -0.5 if typ == "C" else 128.0 ** -0.5
                if hh in (0, 6, 12):
                    if gsc is not None:
                        gsc.__exit__()
                        gsc = None
                    if hh == 0:
                        gsc = self.scope()
                        gsc.__enter__()
                        agk = self.sb(es, "agk", [8, 8, 2048], BF16)
                        agko = self.sb(es, "agko", [8, 2048], BF16)
                        agqo = self.sb(es, "agqo", [8, 2048], BF16)
                    elif hh == 6:
                        gsc = self.scope()
                        gsc.__enter__()
                        bstage = self.sb(es, "bstage", [128, 4, 512], F32)
                        biasb = self.sb(es, "biasb", [128, 8, 512], BF16)
                self.dma("sp", kt[:, :, :], kTf[:, :, hh, :], ("kT_f",), ("kt",))
                self.dma("sp", kto[:, :], self.kT_b[hh * 128:(hh + 1) * 128, :], ("kT_b",), ("kto",))
                for rk in range(8):
                    self.dma("sp", vt[:, rk * 16:(rk + 1) * 16, 0:128], vf[:, rk, hh, :, :], ("v_f",), ("vt",))
                self.dma("sp", vto[:, :, 0:128], vb_[:, hh, :, :], ("v_b",), ("vto",))
                self.dma("sp", qt[:, :], self.qT[hh * 128:(hh + 1) * 128, :], ("qT",), ("qt",))
                if typ == "A":
                    self.dma("sp", agk[0:6, :, :], aK[hh], ("augK",), ("agk",))
                    self.dma("sp", agko[0:6, :], aKo[hh], ("augKo",), ("agko",))
                    self.dma("sp", agqo[0:6, :], aQo[hh], ("augQo",), ("agqo",))
                if typ == "B":
                    hbh = hh - 6
                    for half in range(2):
                        for j4 in range(4):
                            jj = half * 4 + j4
                            srcap = bass.AP(tensor=self.relE_in, offset=(l * 6 + hbh) * ELEN + 896 - 128 * jj,
                                            ap=[[1, 128], [1, 512]])
                            self.dma("sp", bstage[:, j4, :], srcap, (), ("bstage",))
                        sc.add("dve", lambda h, half=half: h.scalar_tensor_tensor(
                            out=biasb[:, half * 4:(half + 1) * 4, :], in0=bstage[:, :, :], scalar=SQ,
                            in1=negB[:, half * 4:(half + 1) * 4, :], op0=ALU.mult, op1=ALU.add),
                            ("bstage", "negB"), ("biasb",))
                for s in range(4):
                    qsl = slice(s * 512, (s + 1) * 512)
                    tiles = []
                    cands = BC[s] if typ == "B" else list(range(MAXU[s]))
                    for ci, u in enumerate(cands):
                        rk, sl = tile_loc(u)
                        col = (128 + s * 8 + ci) if typ == "B" else (s * 32 + u)
                        for jq in range(4):
                            tiles.append((False, rk, sl * 4 + jq, col, jq))
                    for jq in range(4):
                        tiles.append((True, None, s * 4 + jq, 255, jq))
                    nt = len(tiles)
                    ob = 4 if (nfin % 2 == 0 or typ == "C") else 6
                    for ti, (diag, rk, kidx, col, jq) in enumerate(tiles):
                        if diag:
                            kap = lambda lo, hi_, kidx=kidx: kto[lo:hi_, kidx * 128:(kidx + 1) * 128]
                            vap = vto[:, kidx, :]
                            kkeys, vkey = ("kto",), ("vto", "vto1")
                        else:
                            kap = lambda lo, hi_, rk=rk, kidx=kidx: kt[lo:hi_, rk, kidx * 128:(kidx + 1) * 128]
                            vap = vt[:, rk * 16 + kidx, :]
                            kkeys, vkey = ("kt",), ("vt", "vt1")
                        nmap = 2 if typ == "C" else 1
                        sbanks = []
                        for m in range(nmap):
                            bank = nsb % 4
                            nsb += 1
                            sbanks.append(bank)
                            lo, hi_ = (m * 64, m * 64 + 64) if typ == "C" else (0, 128)
                            single = (typ == "C" and not diag)
                            sc.add("pe", lambda h, bank=bank, kap=kap, lo=lo, hi_=hi_, single=single, qsl=qsl: h.matmul(
                                self.ps[bank][:, :], lhsT=kap(lo, hi_), rhs=qt[lo:hi_, qsl], start=True, stop=single),
                                kkeys + ("qt",), (("ps", bank),))
                            if typ == "A":
                                if diag:
                                    sc.add("pe", lambda h, bank=bank, kidx=kidx, qsl=qsl: h.matmul(
                                        self.ps[bank][:, :], lhsT=agko[0:6, kidx * 128:(kidx + 1) * 128], rhs=agqo[0:6, qsl],
                                        start=False, stop=False), ("agko", "agqo"), (("ps", bank),))
                                    sc.add("pe", lambda h, bank=bank, jq=jq: h.matmul(
                                        self.ps[bank][:, :], lhsT=identb, rhs=negC[:, jq, :], start=False, stop=True),
                                        ("cstb", "negC"), (("ps", bank),))
                                else:
                                    sc.add("pe", lambda h, bank=bank, rk=rk, kidx=kidx, qsl=qsl: h.matmul(
                                        self.ps[bank][:, :], lhsT=agk[0:6, rk, kidx * 128:(kidx + 1) * 128], rhs=agqo[0:6, qsl],
                                        start=False, stop=True), ("agk", "agqo"), (("ps", bank),))
                            elif typ == "B":
                                jj = (4 + jq) if diag else jq
                                sc.add("pe", lambda h, bank=bank, jj=jj: h.matmul(
                                    self.ps[bank][:, :], lhsT=self.antiid_b, rhs=biasb[:, jj, :], start=False, stop=True),
                                    ("cstb", "biasb"), (("ps", bank),))
                            elif diag:
                                sc.add("pe", lambda h, bank=bank, jq=jq: h.matmul(
                                    self.ps[bank][:, :], lhsT=identb, rhs=negC[:, 4 + jq, :], start=False, stop=True),
                                    ("cstb", "negC"), (("ps", bank),))
                        pbs = []
                        for m in range(nmap):
                            pb = npt % 6
                            npt += 1
                            pbs.append(pb)
                            sc.add("act", lambda h, pb=pb, bank=sbanks[m], col=col, scale=scale: h.activation(
                                out=pT[pb][:, :], in_=self.ps[bank][:, :], func=AF.Exp, scale=scale,
                                bias=tb[:, col:col + 1]), (("ps", sbanks[m]), "tb"), ("pT%d" % pb,))
                        for m in range(nmap):
                            for qs in range(4):
                                if typ == "C":
                                    obank, ocol = 4 + qs, m * 129
                                else:
                                    obank, ocol = ob + qs // 2, (qs % 2) * 129
                                sc.add("pe", lambda h, pb=pbs[m], qs=qs, obank=obank, ocol=ocol, vap=vap, ti=ti, nt=nt: h.matmul(
                                    self.ps[obank][:, ocol:ocol + 129], lhsT=pT[pb][:, qs * 128:(qs + 1) * 128], rhs=vap,
                                    start=(ti == 0), stop=(ti == nt - 1)), ("pT%d" % pbs[m],) + vkey, (("ps", obank),))
                    grp = 0 if typ == "A" else (1 if typ == "B" else 2)
                    for qs in range(4):
                        fb = nfin % 2
                        if typ == "C":
                            obank, ocol = 4 + qs, 0
                        else:
                            obank, ocol = ob + qs // 2, (qs % 2) * 129
                        f = fin[fb]
                        okey = (("ps", obank),)
                        O0 = self.ps[obank][:, ocol:ocol + 128]
                        sc.add("dve", lambda h, f=f, obank=obank, ocol=ocol: h.reciprocal(
                            out=f[:, 0:1], in_=self.ps[obank][:, ocol + 128:ocol + 129]), okey, ("fin%d" % fb,))
                        sc.add("dve", lambda h, f=f, O0=O0, fb=fb: h.tensor_scalar(
                            out=onb[fb][:, :], in0=O0, scalar1=f[:, 0:1], scalar2=None, op0=ALU.mult),
                            okey + ("fin%d" % fb,), ("onb%d" % fb,))
                        if typ == "C":
                            sc.add("dve", lambda h, f=f, obank=obank: h.reciprocal(
                                out=f[:, 1:2], in_=self.ps[obank][:, 129 + 128:129 + 129]), okey, ("finb%d" % fb,))
                            sc.add("dve", lambda h, f=f: h.tensor_scalar(out=f[:, 1:2], in0=f[:, 1:2], scalar1=nlam[:, 0:1],
                                                                         scalar2=None, op0=ALU.mult),
                                   ("finb%d" % fb, "nlam"), ("finb%d" % fb,))
                            sc.add("dve", lambda h, f=f, obank=obank, fb=fb: h.scalar_tensor_tensor(
                                out=onb[fb][:, :], in0=self.ps[obank][:, 129:129 + 128], scalar=f[:, 1:2], in1=onb[fb][:, :],
                                op0=ALU.mult, op1=ALU.add), okey + ("finb%d" % fb, "onb%d" % fb), ("onb%d" % fb,))
                        sc.add("act", lambda h, f=f, fb=fb: h.activation(out=junk[:, :], in_=onb[fb][:, :], func=AF.Square,
                                                                         accum_out=f[:, 2:3]),
                               ("onb%d" % fb,), ("finc%d" % fb, "ajunk"))
                        sc.add("dve", lambda h, f=f: h.tensor_scalar(out=f[:, 3:4], in0=f[:, 2:3], scalar1=1.0 / 128, scalar2=EPS,
                                                                     op0=ALU.mult, op1=ALU.add), ("finc%d" % fb,), ("find%d" % fb,))
                        sc.add("act", lambda h, f=f: h.activation(out=f[:, 4:5], in_=f[:, 3:4], func=AF.Sqrt),
                               ("find%d" % fb,), ("fine%d" % fb,))
                        sc.add("dve", lambda h, f=f: h.reciprocal(out=f[:, 5:6], in_=f[:, 4:5]), ("fine%d" % fb,), ("finf%d" % fb,))
                        sc.add("dve", lambda h, f=f, fb=fb, grp=grp: h.scalar_tensor_tensor(
                            out=ofb[fb][:, :], in0=onb[fb][:, :], scalar=f[:, 5:6], in1=onw[:, grp, :], op0=ALU.mult, op1=ALU.mult),
                            ("onb%d" % fb, "finf%d" % fb, "onw"), ("ofb%d" % fb,))
                        self.dma("sp", self.o_d[s * 512 + qs * 128:s * 512 + (qs + 1) * 128, hh * 128:(hh + 1) * 128],
                                 ofb[fb][:, :], ("ofb%d" % fb,), ("o_d",))
                        nfin += 1

    def bc_prod(self, es, name, l, cmod, jnorm):
        sc = self.sc
        a = self.load_bc_vec(es, name + "a", self.mod_vec_src(l, cmod), ("mod_f",), name + "a")
        b = self.load_bc_vec(es, name + "b", self.normg_src(l, jnorm), (), name + "b")
        sc.add("pool", lambda h: h.tensor_tensor(out=a[:, :], in0=a[:, :], in1=b[:, :], op=ALU.mult),
               (name + "a", name + "b"), (name,))
        return a

    def o_phase(self, l):
        sc = self.sc
        xsrc = self.x_in if l == 0 else self.out
        with self.scope() as es:
            wo = self.sb(es, "wo", [128, KC, D], BF16)
            self.dma("sp", wo[:, :, :], self.wo_f[l].ap().rearrange("(k p) n -> p k n", p=128), ("wo%d" % l,), ("wo",))
            gm1 = self.bc_prod(es, "gm1", l, 2, 1)
            ot = [self.sb(es, "ot%d" % i, [128, D], F32) for i in range(2)]
            xt = [self.sb(es, "oxt%d" % i, [128, D], F32) for i in range(2)]
            tmp = [self.sb(es, "otmp%d" % i, [128, D], F32) for i in range(2)]
            oT = [self.sb(es, "oT%d" % i, [128, KC, 128], BF16) for i in range(2)]
            st = [self.sb(es, "ost%d" % i, [128, 8], F32) for i in range(2)]
            junk = self.sb(es, "ojunk", [128, 512], BF16)
            for tt in range(16):
                b = tt % 2
                rows = slice(tt * 128, (tt + 1) * 128)
                self.dma("sp", ot[b][:, :], self.o_d[rows, :], ("o_d",), ("ot%d" % b,))
                self.dma("sp", xt[b][:, :], xsrc[rows, :], (("out", tt),) if l else (), ("oxt%d" % b,))
                for q4 in range(4):
                    bank = q4
                    for j_ in range(4):
                        kc = q4 * 4 + j_
                        sc.add("pe", lambda h, b=b, kc=kc, j_=j_, bank=bank: h.transpose(
                            out=self.ps[bank][:, j_ * 128:(j_ + 1) * 128], in_=ot[b][:, kc * 128:(kc + 1) * 128],
                            identity=self.ident), ("ot%d" % b, "cst"), (("ps", bank),))
                    if q4 % 2 == 0:
                        sc.add("act", lambda h, b=b, q4=q4, bank=bank: h.activation(
                            out=oT[b][:, q4 * 4:(q4 + 1) * 4, :], in_=self.ps[bank][:, :].rearrange("p (a c) -> p a c", a=4),
                            func=AF.Copy), (("ps", bank),), (("oT", b, q4),))
                    else:
                        sc.add("dve", lambda h, b=b, q4=q4, bank=bank: h.tensor_copy(
                            out=oT[b][:, q4 * 4:(q4 + 1) * 4, :], in_=self.ps[bank][:, :].rearrange("p (a c) -> p a c", a=4)),
                            (("ps", bank),), (("oT", b, q4),))
                okeys = tuple(("oT", b, q4) for q4 in range(4))
                for dq in range(4):
                    bank = 4 + dq
                    for kc in range(KC):
                        sc.add("pe", lambda h, b=b, kc=kc, dq=dq, bank=bank: h.matmul(
                            self.ps[bank][:, :], lhsT=oT[b][:, kc, :], rhs=wo[:, kc, dq * 512:(dq + 1) * 512],
                            start=(kc == 0), stop=(kc == KC - 1)), okeys + ("wo",), (("ps", bank),))
                    sc.add("act", lambda h, b=b, dq=dq, bank=bank: h.activation(
                        out=junk[:, :], in_=self.ps[bank][:, :], func=AF.Square, accum_out=st[b][:, dq:dq + 1]),
                        (("ps", bank),), (("ost", b, dq), "ojunk"))
                sc.add("dve", lambda h, b=b: h.reduce_sum(out=st[b][:, 4:5], in_=st[b][:, 0:4], axis=AX.X),
                       tuple(("ost", b, dq) for dq in range(4)), (("ost4", b),))
                sc.add("dve", lambda h, b=b: h.tensor_scalar(out=st[b][:, 5:6], in0=st[b][:, 4:5], scalar1=1.0 / D, scalar2=EPS,
                                                             op0=ALU.mult, op1=ALU.add), (("ost4", b),), (("ost5", b),))
                sc.add("act", lambda h, b=b: h.activation(out=st[b][:, 6:7], in_=st[b][:, 5:6], func=AF.Sqrt),
                       (("ost5", b),), (("ost6", b),))
                sc.add("dve", lambda h, b=b: h.reciprocal(out=st[b][:, 7:8], in_=st[b][:, 6:7]), (("ost6", b),), (("ost7", b),))
                for dq in range(4):
                    bank = 4 + dq
                    cs = slice(dq * 512, (dq + 1) * 512)
                    sc.add("dve", lambda h, b=b, bank=bank, cs=cs: h.scalar_tensor_tensor(
                        out=tmp[b][:, cs], in0=self.ps[bank][:, :], scalar=st[b][:, 7:8], in1=gm1[:, cs],
                        op0=ALU.mult, op1=ALU.mult), (("ps", bank), ("ost7", b), "gm1"), (("otmp", b, dq),))
                    sc.add("pool", lambda h, b=b, cs=cs: h.tensor_tensor(out=xt[b][:, cs], in0=tmp[b][:, cs], in1=xt[b][:, cs],
                                                                         op=ALU.add),
                           (("otmp", b, dq), "oxt%d" % b), ("oxt%d" % b,))
                self.dma("sp", self.out[rows, :], xt[b][:, :], ("oxt%d" % b,), (("out", tt),))

    def gating(self, lg, comb):
        sc = self.sc
        with self.scope() as es:
            m = [self.sb(es, "gm%d" % i, [128, 8], F32) for i in range(2)]
            k1 = [self.sb(es, "gk1%d" % i, [128, 8], F32) for i in range(2)]
            k2 = [self.sb(es, "gk2%d" % i, [128, 8], F32) for i in range(2)]
            l2 = [self.sb(es, "gl2%d" % i, [128, 8], F32) for i in range(2)]
            for tt in range(16):
                b = tt % 2
                M, K1, K2, L2 = m[b], k1[b], k2[b], l2[b]
                kb = "g%d" % b
                sc.add("dve", lambda h, M=M, tt=tt: h.reduce_max(out=M[:, 0:1], in_=lg[:, tt, :], axis=AX.X),
                       (("lg", tt),), (kb + "m1",))
                sc.add("dve", lambda h, M=M, K1=K1, tt=tt: h.tensor_scalar(out=K1[:, :], in0=lg[:, tt, :], scalar1=M[:, 0:1],
                                                                           scalar2=None, op0=ALU.is_equal),
                       (("lg", tt), kb + "m1"), (kb + "k1",))
                sc.add("dve", lambda h, K1=K1, L2=L2, tt=tt: h.scalar_tensor_tensor(
                    out=L2[:, :], in0=K1[:, :], scalar=-1.0e30, in1=lg[:, tt, :], op0=ALU.mult, op1=ALU.add),
                    (kb + "k1", ("lg", tt)), (kb + "l2",))
                sc.add("dve", lambda h, M=M, L2=L2: h.reduce_max(out=M[:, 1:2], in_=L2[:, :], axis=AX.X),
                       (kb + "l2",), (kb + "m2",))
                sc.add("dve", lambda h, M=M, K2=K2, L2=L2: h.tensor_scalar(out=K2[:, :], in0=L2[:, :], scalar1=M[:, 1:2],
                                                                           scalar2=None, op0=ALU.is_equal),
                       (kb + "l2", kb + "m2"), (kb + "k2",))
                sc.add("dve", lambda h, M=M: h.tensor_tensor(out=M[:, 2:3], in0=M[:, 1:2], in1=M[:, 0:1], op=ALU.subtract),
                       (kb + "m1", kb + "m2"), (kb + "d",))
                sc.add("act", lambda h, M=M: h.activation(out=M[:, 3:4], in_=M[:, 2:3], func=AF.Exp), (kb + "d",), (kb + "e",))
                sc.add("dve", lambda h, M=M: h.tensor_scalar(out=M[:, 4:5], in0=M[:, 3:4], scalar1=1.0, scalar2=None,
                                                             op0=ALU.add), (kb + "e",), (kb + "den",))
                sc.add("dve", lambda h, M=M: h.reciprocal(out=M[:, 5:6], in_=M[:, 4:5]), (kb + "den",), (kb + "g1",))
                sc.add("dve", lambda h, M=M: h.tensor_tensor(out=M[:, 6:7], in0=M[:, 3:4], in1=M[:, 5:6], op=ALU.mult),
                       (kb + "e", kb + "g1"), (kb + "g2",))
                sc.add("dve", lambda h, M=M, K1=K1: h.tensor_scalar(out=K1[:, :], in0=K1[:, :], scalar1=M[:, 5:6], scalar2=None,
                                                                    op0=ALU.mult), (kb + "k1", kb + "g1"), (kb + "k1",))
                sc.add("dve", lambda h, M=M, K1=K1, K2=K2, tt=tt: h.scalar_tensor_tensor(
                    out=comb[:, tt, :], in0=K2[:, :], scalar=M[:, 6:7], in1=K1[:, :], op0=ALU.mult, op1=ALU.add),
                    (kb + "k2", kb + "g2", kb + "k1"), (("comb", tt),))

    def ffn_phase(self, l, wsets, comb):
        sc = self.sc
        ne = len(wsets)
        hTv = self.hT_d.ap().rearrange("p (k t) -> p k t", k=KC)
        with self.scope() as es:
            gf3 = self.bc_prod(es, "gf3", l, 5, 3)
            hTt = self.sb(es, "hTt", [128, KC, 512], BF16)
            gT = self.sb(es, "gT", [128, FC, 512], BF16)
            yacc = self.sb(es, "yacc", [128, 4, D], F32)
            wgb = [self.sb(es, "wgb%d" % i, [128, KC, 256], BF16) for i in range(2)]
            wub = [self.sb(es, "wub%d" % i, [128, KC, 256], BF16) for i in range(2)]
            wdb = [self.sb(es, "wdb%d" % i, [128, 512], BF16) for i in range(4)]
            slt = [self.sb(es, "slt%d" % i, [128, 512], F32) for i in range(2)]
            xt = self.sb(es, "fxt", [128, D], F32)
            junk = self.sb(es, "fjunk", [128, D], BF16)
            st = self.sb(es, "fst", [128, 4], F32)
            nw = 0
            nwd = 0
            nsl = 0
            npb = 0
            for t4 in range(4):
                self.dma("sp", hTt[:, :, :], hTv[:, :, t4 * 512:(t4 + 1) * 512], ("hT_d",), ("hTt",))
                for e in range(ne):
                    wg, wu, wd, rk = wsets[e]
                    wgv = wg.ap().rearrange("(e k p) n -> e p k n", p=128, k=KC)[e if ne > 1 else 0]
                    wuv = wu.ap().rearrange("(e k p) n -> e p k n", p=128, k=KC)[e if ne > 1 else 0]
                    wdv = wd.ap().rearrange("(e f p) n -> e f p n", p=128, f=FC)[e if ne > 1 else 0]
                    for fg in range(FC // 2):
                        wb = nw % 2
                        nw += 1
                        self.dma("sp", wgb[wb][:, :, :], wgv[:, :, fg * 256:(fg + 1) * 256], rk, ("wgb%d" % wb,))
                        self.dma("sp", wub[wb][:, :, :], wuv[:, :, fg * 256:(fg + 1) * 256], rk, ("wub%d" % wb,))
                        for half in range(2):
                            fc = fg * 2 + half
                            ba, bu = (0, 1) if npb % 2 == 0 else (2, 3)
                            npb += 1
                            for k in range(KC):
                                sc.add("pe", lambda h, k=k, wb=wb, half=half, ba=ba: h.matmul(
                                    self.ps[ba][:, :], lhsT=wgb[wb][:, k, half * 128:(half + 1) * 128], rhs=hTt[:, k, :],
                                    start=(k == 0), stop=(k == KC - 1)), ("wgb%d" % wb, "hTt"), (("ps", ba),))
                            for k in range(KC):
                                sc.add("pe", lambda h, k=k, wb=wb, half=half, bu=bu: h.matmul(
                                    self.ps[bu][:, :], lhsT=wub[wb][:, k, half * 128:(half + 1) * 128], rhs=hTt[:, k, :],
                                    start=(k == 0), stop=(k == KC - 1)), ("wub%d" % wb, "hTt"), (("ps", bu),))
                            sb_ = nsl % 2
                            nsl += 1
                            sc.add("act", lambda h, sb_=sb_, ba=ba: h.activation(out=slt[sb_][:, :], in_=self.ps[ba][:, :],
                                                                                func=AF.Silu), (("ps", ba),), ("slt%d" % sb_,))
                            sc.add("dve", lambda h, sb_=sb_, bu=bu, fc=fc: h.tensor_tensor(
                                out=gT[:, fc, :], in0=slt[sb_][:, :], in1=self.ps[bu][:, :], op=ALU.mult),
                                ("slt%d" % sb_, ("ps", bu)), (("gT", fc),))
                    gkeys = tuple(("gT", fc) for fc in range(FC))
                    for dq in range(4):
                        for fc in range(FC):
                            db = nwd % 4
                            nwd += 1
                            self.dma("sp", wdb[db][:, :], wdv[fc][:, dq * 512:(dq + 1) * 512], rk, ("wdb%d" % db,))
                            for sub in range(4):
                                sc.add("pe", lambda h, fc=fc, sub=sub, db=db: h.matmul(
                                    self.ps[4 + sub][:, :], lhsT=gT[:, fc, sub * 128:(sub + 1) * 128], rhs=wdb[db][:, :],
                                    start=(fc == 0), stop=(fc == FC - 1)), gkeys + ("wdb%d" % db,), (("ps", 4 + sub),))
                        cs = slice(dq * 512, (dq + 1) * 512)
                        for sub in range(4):
                            tt = t4 * 4 + sub
                            if comb is None:
                                sc.add("act", lambda h, sub=sub, cs=cs: h.activation(out=yacc[:, sub, cs], in_=self.ps[4 + sub][:, :],
                                                                                    func=AF.Copy),
                                       (("ps", 4 + sub),), (("yacc", sub, dq),))
                            elif e == 0:
                                sc.add("dve", lambda h, sub=sub, cs=cs, tt=tt, e=e: h.tensor_scalar(
                                    out=yacc[:, sub, cs], in0=self.ps[4 + sub][:, :], scalar1=comb[:, tt, e:e + 1], scalar2=None,
                                    op0=ALU.mult), (("ps", 4 + sub), ("comb", tt)), (("yacc", sub, dq),))
                            else:
                                sc.add("dve", lambda h, sub=sub, cs=cs, tt=tt, e=e: h.scalar_tensor_tensor(
                                    out=yacc[:, sub, cs], in0=self.ps[4 + sub][:, :], scalar=comb[:, tt, e:e + 1],
                                    in1=yacc[:, sub, cs], op0=ALU.mult, op1=ALU.add),
                                    (("ps", 4 + sub), ("comb", tt), ("yacc", sub, dq)), (("yacc", sub, dq),))
                for sub in range(4):
                    tt = t4 * 4 + sub
                    rows = slice(tt * 128, (tt + 1) * 128)
                    yk = tuple(("yacc", sub, dq) for dq in range(4))
                    self.dma("sp", xt[:, :], self.out[rows, :], (("out", tt),), ("fxt",))
                    sc.add("act", lambda h, sub=sub: h.activation(out=junk[:, :], in_=yacc[:, sub, :], func=AF.Square,
                                                                  accum_out=st[:, 0:1]), yk, ("fst0", "fjunk"))
                    sc.add("dve", lambda h: h.tensor_scalar(out=st[:, 1:2], in0=st[:, 0:1], scalar1=1.0 / D, scalar2=EPS,
                                                             op0=ALU.mult, op1=ALU.add), ("fst0",), ("fst1",))
                    sc.add("act", lambda h: h.activation(out=st[:, 2:3], in_=st[:, 1:2], func=AF.Sqrt), ("fst1",), ("fst2",))
                    sc.add("dve", lambda h: h.reciprocal(out=st[:, 3:4], in_=st[:, 2:3]), ("fst2",), ("fst3",))
                    sc.add("dve", lambda h, sub=sub: h.scalar_tensor_tensor(
                        out=yacc[:, sub, :], in0=yacc[:, sub, :], scalar=st[:, 3:4], in1=gf3[:, :], op0=ALU.mult, op1=ALU.mult),
                        yk + ("fst3", "gf3"), yk)
                    sc.add("pool", lambda h, sub=sub: h.tensor_tensor(out=xt[:, :], in0=yacc[:, sub, :], in1=xt[:, :], op=ALU.add),
                           yk + ("fxt",), ("fxt",))
                    self.dma("sp", self.out[rows, :], xt[:, :], ("fxt",), (("out", tt),))

    def layer(self, l):
        sc = self.sc
        xsrc = self.x_in if l == 0 else self.out
        with self.scope() as es:
            hT = self.sb(es, "hT", [128, KC, TOK], BF16)
            self.norm_to_hT(l, 0, hT, lambda tt: (xsrc[tt * 128:(tt + 1) * 128, :], (("out", tt),) if l else ()))
            self.proj_phase(l, hT)
        if self.stop_after == "P%d" % l:
            self.tap("qT", self.qT, ("qT",))
            self.tap("kT", self.kT_f, ("kT_f",))
            self.tap("v", self.v_f, ("v_f",))
            self.tap("g", self.g_f, ("g_f",))
            self.done = True
            return
        if l == 0 and self.ewg_in is not None:
            self.prep_experts()
        self.f_phase(l)
        self.attn_phase(l)
        if self.stop_after == "A%d" % l:
            self.tap("o", self.o_d, ("o_d",))
            self.done = True
            return
        self.o_phase(l)
        if self.stop_after == "O%d" % l:
            self.done = True
            return
        with self.scope() as es:
            comb = None
            if l == 1:
                lg = self.sb(es, "lg", [128, 16, 8], F32)
                comb = self.sb(es, "comb", [128, 16, 8], F32)
            with self.scope() as es2:
                hT = self.sb(es2, "hT2", [128, KC, TOK], BF16)
                self.norm_to_hT(l, 1, hT, lambda tt: (self.out[tt * 128:(tt + 1) * 128, :], (("out", tt),)),
                                router=({"lg": lg} if l == 1 else None))
                self.dma("sp", self.hT_d.ap().rearrange("p (k t) -> p k t", k=KC), hT[:, :, :],
                         tuple(("hT", tt) for tt in range(16)), ("hT_d",))
            if l == 1:
                self.gating(lg, comb)
                if self.stop_after == "G1":
                    cd = self.nc.dram_tensor("tap_comb", [128, 128], F32, kind="ExternalOutput")
                    self.dma("sp", cd.ap(), comb[:, :, :].rearrange("p a b -> p (a b)"), tuple(("comb", tt) for tt in range(16)), ("out",))
                    ld = self.nc.dram_tensor("tap_lg", [128, 128], F32, kind="ExternalOutput")
                    self.dma("sp", ld.ap(), lg[:, :, :].rearrange("p a b -> p (a b)"), tuple(("lg", tt) for tt in range(16)), ("out",))
                    self.done = True
                    return
                wsets = [(self.ew_f[0], self.ew_f[1], self.ew_f[2], ("ew0", "ew1", "ew2")) for e in range(NE)]
            else:
                wsets = [(self.fw_f[0], self.fw_f[1], self.fw_f[2], ("fw0", "fw1", "fw2"))]
            self.ffn_phase(l, wsets, comb)
        if self.stop_after == "F%d" % l:
            if l == 0:
                self.tap("o", self.o_d, ("o_d",))
            self.done = True


def fm_cols():
    cols = []
    WA = 768
    qa, ka, va, fa = 0, WA, 2 * WA, 3 * WA
    qb = 3 * WA + 6
    kb, vb = qb + 768, qb + 1536
    qc = qb + 3 * 768
    kc_, vc = qc + 512, qc + 1024
    r = np.arange(128)
    perm = r.copy()
    for base in (0, 64):
        perm[base:base + 8] = np.arange(base + 8, base + 16)
        perm[base + 8:base + 16] = np.arange(base, base + 8)
    for h in range(6):
        cols += list(qa + h * 128 + r) + list(ka + h * 128 + r)
    for h in range(6):
        cols += list(qb + h * 128 + r) + list(kb + h * 128 + r)
    for h in range(4):
        cols += list(qc + h * 128 + r) + list(qc + h * 128 + perm) + list(kc_ + h * 128 + r) + list(kc_ + h * 128 + perm)
    vcols = list(va + np.arange(768)) + list(vb + np.arange(768)) + list(vc + np.arange(512))
    gcols = list(fa + np.arange(6))
    return np.array(cols), np.array(vcols), np.array(gcols)


def host_consts():
    cst = np.zeros((128, 512), np.float32)
    cst[:, 0:128] = np.eye(128)
    cst[:, 128:256] = np.eye(128)[::-1]
    p = np.arange(128)
    half = 8
    inv = (500000.0 ** (-(np.arange(half, dtype=np.float32) * 2.0 / 16))).astype(np.float32)
    invp = np.zeros(128, np.float32)
    sgn = np.zeros(128, np.float32)
    for base in (0, 64):
        invp[base:base + 8] = inv
        invp[base + 8:base + 16] = inv
        sgn[base:base + 8] = -1.0
        sgn[base + 8:base + 16] = 1.0
    cst[:, 384] = invp
    cst[:, 385] = sgn
    cst[:, 387] = 1.0
    k = np.arange(128)[:, None]
    q = np.arange(512)[None, :]
    cm = np.zeros((128, 8, 512), np.float32)
    for jj in range(4):
        cm[:, jj] = ((jj * 128 + k) <= q)
        cm[:, 4 + jj] = ((jj * 128 + k) // 64 <= q // 64)
    bm = np.zeros((128, 8, 512), np.float32)
    for jj in range(8):
        srel = 128 * jj + k - 512
        c64 = (q // 64) * 64
        bm[:, jj] = (srel >= c64 - 512) & (srel < c64 + 64)
    bm = bm[::-1].copy()
    return cst, cm.reshape(128, -1), bm.reshape(128, -1)


def core_tb(r):
    tb = np.zeros((128, 256), np.float32)
    tiles = own_tiles(r)
    BC = [list(range(0, 7)), list(range(7, 15)), list(range(15, 23)), list(range(23, 31))]
    for s in range(4):
        g = tiles[s]
        for u in range(32):
            tb[:, s * 32 + u] = 0.0 if u < g else -60000.0
        for ci, u in enumerate(BC[s]):
            tb[:, 128 + s * 8 + ci] = 0.0 if u == g - 1 else -60000.0
    return tb


def core_sel(r):
    sel = np.zeros((8, 128), np.float32)
    for s, g in enumerate(own_tiles(r)):
        sel[:, s * 32 + g] = 1.0
    return sel


def make_in_maps(inp):
    x = np.asarray(inp["x"])[0]
    pos = np.asarray(inp["positions"])[0]
    fmc, vcs, gcs = fm_cols()
    w_in = np.asarray(inp["w_in"])
    cst, cm, bm = host_consts()
    idx = np.clip(np.arange(ELEN) - 511, -REL_CLIP, REL_CLIP) + REL_CLIP
    relE = np.ascontiguousarray(np.asarray(inp["rel_bias"])[:, :, idx])
    wgate = np.zeros((2, D, 8), np.float32)
    wgate[:, :, :6] = w_in[:, :, gcs]
    bfp = np.zeros((2, 8), np.float32)
    bfp[:, :6] = np.asarray(inp["b_f"])
    mod_w = np.asarray(inp["mod_w"]).reshape(2, D, 6, 8, 256)
    mod_b = np.asarray(inp["mod_b"]).reshape(2, 6, 8, 256)
    maps = []
    for r in range(NCORES):
        tiles = own_tiles(r)
        rows = np.concatenate([np.arange(g * 512, (g + 1) * 512) for g in tiles])
        rs = slice(r * 256, (r + 1) * 256)
        m = {
            "x": np.ascontiguousarray(x[rows]),
            "pos": np.ascontiguousarray(pos[rows]).reshape(1, TOK).astype(np.int32),
            "c": np.asarray(inp["c"]).reshape(D, 1),
            "modw": np.ascontiguousarray(mod_w[:, :, :, r, :]).reshape(2, D, 1536 * 0 + 6 * 256),
            "modb": np.ascontiguousarray(mod_b[:, :, r, :]).reshape(1, 2 * 6 * 256),
            "normg": np.asarray(inp["norm_g"]).reshape(8, D),
            "wfm": np.ascontiguousarray(w_in[:, rs][:, :, fmc]),
            "wv": np.ascontiguousarray(w_in[:, rs][:, :, vcs]),
            "wgate": wgate, "bf": bfp, "relE": relE,
            "lam": np.asarray(inp["lam"]).reshape(2, 256),
            "onorm": np.asarray(inp["onorm"]),
            "wo": np.ascontiguousarray(np.asarray(inp["w_o"])[:, rs]),
            "fwg": np.ascontiguousarray(np.asarray(inp["ffn_wg"])[0, rs]),
            "fwu": np.ascontiguousarray(np.asarray(inp["ffn_wu"])[0, rs]),
            "fwd": np.ascontiguousarray(np.asarray(inp["ffn_wd"])[0, r * 896:(r + 1) * 896]),
            "rw": np.asarray(inp["router_w"])[0], "rb": np.asarray(inp["router_b"]).reshape(1, 8),
            "ewg": np.asarray(inp["exp_wg"])[0, r], "ewu": np.asarray(inp["exp_wu"])[0, r],
            "ewd": np.asarray(inp["exp_wd"])[0, r],
            "cst": cst, "cmask": cm, "bmask": bm, "tb": core_tb(r), "sel": core_sel(r),
        }
        maps.append(m)
    return maps


def kernel(**inp):
    b = Builder()
    nc = b.build()
    maps = make_in_maps(inp)
    res = run_bass_kernel_spmd(nc, maps, core_ids=list(range(NCORES)))
    out = np.zeros((1, S, D), np.float32)
    for r in range(NCORES):
        o = np.asarray(res.results[r]["out"])
        for s_, g in enumerate(own_tiles(r)):
            out[0, g * 512:(g + 1) * 512] = o[s_ * 512:(s_ + 1) * 512]
    return out
```
